# Optimizing a Trainium2 kernel written in Bass

```python
import math
import jax, jax.numpy as jnp
from jax import lax
import numpy as np


D_MODEL = 1024
BATCH = 16
SEQ = 2048
DEPTH = 2

MEM_TOKENS = 256
HEAD_DIM = 64
MIX_WIDTH = D_MODEL
MLA_WIDTH = MIX_WIDTH // 2
MEM_WIDTH = MIX_WIDTH // 4
CONV_CH = MIX_WIDTH - MLA_WIDTH - MEM_WIDTH
N_MLA_HEADS = MLA_WIDTH // HEAD_DIM
N_MEM_HEADS = MEM_WIDTH // HEAD_DIM
QK_NOPE_DIM = 64
QK_ROPE_DIM = 32
QK_HEAD_DIM = QK_NOPE_DIM + QK_ROPE_DIM
V_HEAD_DIM = HEAD_DIM
Q_LORA_RANK = D_MODEL // 4
KV_LORA_RANK = D_MODEL // 8
CONV_K = 3
IN_SPLITS = (Q_LORA_RANK, KV_LORA_RANK, QK_ROPE_DIM, MEM_WIDTH, CONV_CH, CONV_CH, CONV_CH)
IN_PROJ_WIDTH = sum(IN_SPLITS)
D_FF = 2816
N_EXPERTS = 8
TOP_K = 2
EXPERT_FF = D_FF // TOP_K
N_DENSE = (DEPTH + 1) // 2
N_MOE = DEPTH // 2
Q_BLOCK = 128
ROPE_THETA = 10000.0
EPS = 1e-6

kernel_name = 'hybrid_mla_mem_shortconv_moe'


def rms_norm(x, g):
    xf = x.astype(jnp.float32)
    y = xf * lax.rsqrt(jnp.mean(xf * xf, axis=-1, keepdims=True) + EPS)
    return (y * g.astype(jnp.float32)).astype(x.dtype)


def rope_tables(seq, dtype):
    inv = 1.0 / (ROPE_THETA ** (jnp.arange(0, QK_ROPE_DIM, 2, dtype=jnp.float32) / QK_ROPE_DIM))
    ang = jnp.arange(seq, dtype=jnp.float32)[:, None] * inv[None, :]
    return jnp.cos(ang)[:, None, :].astype(dtype), jnp.sin(ang)[:, None, :].astype(dtype)


def apply_rope(x, cos, sin):
    x1, x2 = jnp.split(x, 2, axis=-1)
    return jnp.concatenate([x1 * cos - x2 * sin, x2 * cos + x1 * sin], axis=-1)


def causal_block_attention(q, k, v):
    B, S, H, Dq = q.shape
    nb = S // Q_BLOCK
    scale = 1.0 / math.sqrt(Dq)
    qb = q.reshape(B, nb, Q_BLOCK, H, Dq).transpose(1, 0, 2, 3, 4)
    kpos = jnp.arange(S)
    neg = jnp.finfo(jnp.float32).min

    def block(args):
        qi, start = args
        s = jnp.einsum('bqhd,bkhd->bhqk', qi, k).astype(jnp.float32) * scale
        qpos = start + jnp.arange(Q_BLOCK)
        s = jnp.where(kpos[None, :] <= qpos[:, None], s, neg)
        p = jax.nn.softmax(s, axis=-1).astype(v.dtype)
        return jnp.einsum('bhqk,bkhd->bqhd', p, v)

    out = lax.map(block, (qb, jnp.arange(nb) * Q_BLOCK))
    return out.transpose(1, 0, 2, 3, 4).reshape(B, S, H, v.shape[-1])


def causal_short_conv(u, w):
    C = u.shape[-1]
    return lax.conv_general_dilated(u, w[:, None, :], window_strides=(1,), padding=[(CONV_K - 1, 0)],
                                    dimension_numbers=('NWC', 'WIO', 'NWC'), feature_group_count=C)


def swiglu(h, w_gu, w_down):
    g, u = jnp.split(h @ w_gu, 2, axis=-1)
    return (jax.nn.silu(g) * u) @ w_down


def moe_swiglu(h, w_router, w_gu, w_down):
    logits = (h @ w_router).astype(jnp.float32)
    top_val, top_idx = lax.top_k(logits, TOP_K)
    top_w = jax.nn.softmax(top_val, axis=-1)
    combine = jnp.einsum('bsk,bske->bse', top_w,
                         jax.nn.one_hot(top_idx, N_EXPERTS, dtype=jnp.float32)).astype(h.dtype)
    y = jnp.zeros_like(h)
    for e in range(N_EXPERTS):
        y = y + combine[..., e:e + 1] * swiglu(h, w_gu[e], w_down[e])
    return y


def hybrid_mixer(h, mem_n, cos, sin, w_in, g_q_lat, w_q_up, g_kv_lat, w_kv_up, g_q_mla, g_k_mla,
                 w_mem_kv, g_q_mem, g_k_mem, conv_w, g_out, w_out):
    B, S, _ = h.shape
    H = N_MLA_HEADS
    z = h @ w_in
    offs = np.cumsum(IN_SPLITS)[:-1].tolist()
    q_lat, kv_lat, k_pe, q_mem, gate_b, gate_c, u = jnp.split(z, offs, axis=-1)

    q = (rms_norm(q_lat, g_q_lat) @ w_q_up).reshape(B, S, H, QK_HEAD_DIM)
    kv = (rms_norm(kv_lat, g_kv_lat) @ w_kv_up).reshape(B, S, H, QK_NOPE_DIM + V_HEAD_DIM)
    k_nope, v = kv[..., :QK_NOPE_DIM], kv[..., QK_NOPE_DIM:]
    q_nope = rms_norm(q[..., :QK_NOPE_DIM], g_q_mla[:QK_NOPE_DIM])
    q_pe = apply_rope(rms_norm(q[..., QK_NOPE_DIM:], g_q_mla[QK_NOPE_DIM:]), cos, sin)
    k_nope = rms_norm(k_nope, g_k_mla[:QK_NOPE_DIM])
    k_pe = apply_rope(rms_norm(k_pe, g_k_mla[QK_NOPE_DIM:])[:, :, None, :], cos, sin)
    qf = jnp.concatenate([q_nope, q_pe], axis=-1)
    kf = jnp.concatenate([k_nope, jnp.broadcast_to(k_pe, (B, S, H, QK_ROPE_DIM))], axis=-1)
    o_mla = causal_block_attention(qf, kf, v).reshape(B, S, MLA_WIDTH)

    km, vm = jnp.split(mem_n @ w_mem_kv, 2, axis=-1)
    M = mem_n.shape[1]
    km = rms_norm(km.reshape(B, M, N_MEM_HEADS, HEAD_DIM), g_k_mem)
    vm = vm.reshape(B, M, N_MEM_HEADS, HEAD_DIM)
    qm = rms_norm(q_mem.reshape(B, S, N_MEM_HEADS, HEAD_DIM), g_q_mem)
    s = jnp.einsum('bshd,bmhd->bhsm', qm, km).astype(jnp.float32) * (1.0 / math.sqrt(HEAD_DIM))
    p = jax.nn.softmax(s, axis=-1).astype(vm.dtype)
    o_mem = jnp.einsum('bhsm,bmhd->bshd', p, vm).reshape(B, S, MEM_WIDTH)

    o_conv = gate_b * causal_short_conv(gate_c * u, conv_w)

    a, b = MLA_WIDTH, MLA_WIDTH + MEM_WIDTH
    o = jnp.concatenate([rms_norm(o_mla, g_out[:a]), rms_norm(o_mem, g_out[a:b]),
                         rms_norm(o_conv, g_out[b:])], axis=-1)
    return o @ w_out


def setup_inputs(seed: int = 0) -> dict:
    key = jax.random.key(seed)
    ks = jax.random.split(key, 24)
    f32 = jnp.float32

    def nrm(k, shape, fan_in):
        return jax.random.normal(k, shape, f32) * (fan_in ** -0.5)

    def gain(k, shape):
        return 1.0 + 0.02 * jax.random.normal(k, shape, f32)

    L = DEPTH
    return {
        'x': jax.random.normal(ks[0], (BATCH, SEQ, D_MODEL), f32),
        'mem': jax.random.normal(ks[1], (BATCH, MEM_TOKENS, D_MODEL), f32),
        'g_mix': gain(ks[2], (L, D_MODEL)),
        'w_in': nrm(ks[3], (L, D_MODEL, IN_PROJ_WIDTH), D_MODEL),
        'g_q_lat': gain(ks[4], (L, Q_LORA_RANK)),
        'w_q_up': nrm(ks[5], (L, Q_LORA_RANK, N_MLA_HEADS * QK_HEAD_DIM), Q_LORA_RANK),
        'g_kv_lat': gain(ks[6], (L, KV_LORA_RANK)),
        'w_kv_up': nrm(ks[7], (L, KV_LORA_RANK, N_MLA_HEADS * (QK_NOPE_DIM + V_HEAD_DIM)), KV_LORA_RANK),
        'g_q_mla': gain(ks[8], (L, QK_HEAD_DIM)),
        'g_k_mla': gain(ks[9], (L, QK_HEAD_DIM)),
        'g_mem': gain(ks[10], (L, D_MODEL)),
        'w_mem_kv': nrm(ks[11], (L, D_MODEL, 2 * MEM_WIDTH), D_MODEL),
        'g_q_mem': gain(ks[12], (L, HEAD_DIM)),
        'g_k_mem': gain(ks[13], (L, HEAD_DIM)),
        'conv_w': nrm(ks[14], (L, CONV_K, CONV_CH), CONV_K),
        'g_out': gain(ks[15], (L, MIX_WIDTH)),
        'w_out': nrm(ks[16], (L, MIX_WIDTH, D_MODEL), MIX_WIDTH),
        'g_ffn': gain(ks[17], (L, D_MODEL)),
        'w_dense_gu': nrm(ks[18], (N_DENSE, D_MODEL, 2 * D_FF), D_MODEL),
        'w_dense_down': nrm(ks[19], (N_DENSE, D_FF, D_MODEL), D_FF),
        'w_router': nrm(ks[20], (N_MOE, D_MODEL, N_EXPERTS), D_MODEL),
        'w_expert_gu': nrm(ks[21], (N_MOE, N_EXPERTS, D_MODEL, 2 * EXPERT_FF), D_MODEL),
        'w_expert_down': nrm(ks[22], (N_MOE, N_EXPERTS, EXPERT_FF, D_MODEL), EXPERT_FF),
    }


def reference(x, mem, g_mix, w_in, g_q_lat, w_q_up, g_kv_lat, w_kv_up, g_q_mla, g_k_mla, g_mem,
              w_mem_kv, g_q_mem, g_k_mem, conv_w, g_out, w_out, g_ffn, w_dense_gu, w_dense_down,
              w_router, w_expert_gu, w_expert_down):
    cos, sin = rope_tables(x.shape[1], x.dtype)
    for l in range(DEPTH):
        h = rms_norm(x, g_mix[l])
        mem_n = rms_norm(mem, g_mem[l])
        x = x + hybrid_mixer(h, mem_n, cos, sin, w_in[l], g_q_lat[l], w_q_up[l], g_kv_lat[l], w_kv_up[l],
                             g_q_mla[l], g_k_mla[l], w_mem_kv[l], g_q_mem[l], g_k_mem[l], conv_w[l],
                             g_out[l], w_out[l])
        h = rms_norm(x, g_ffn[l])
        if l % 2 == 0:
            x = x + swiglu(h, w_dense_gu[l // 2], w_dense_down[l // 2])
        else:
            x = x + moe_swiglu(h, w_router[l // 2], w_expert_gu[l // 2], w_expert_down[l // 2])
    return x
```

```python
import math
import numpy as np
import concourse.bass as bass
import concourse.mybir as mybir
from concourse.bass_utils import run_bass_kernel_spmd

F32 = mybir.dt.float32
BF16 = mybir.dt.bfloat16
ALU = mybir.AluOpType
AF = mybir.ActivationFunctionType
AX = mybir.AxisListType

NCORES = 8
SEQ_PER_CORE = 2
S = 2048
D = 1024
KC = 8
TB = 512
NTB = S // TB
MEMT = 256
EPS = 1e-6
NH = 8
NMH = 4
EFF = 1408
NJ = EFF // 128
WIN_COLS = 1472
WQ_COLS = 1536

G_MIX, G_FFN, G_QLAT, G_KVLAT, G_Q96, G_Q96SW, G_K96, G_K96SW, G_QMEM, G_KMEM = 0, 8, 16, 18, 19, 20, 21, 22, 23, 24
G_OMLA, G_OMEM, G_OCONV, G_CONVW, G_MEM = 25, 33, 37, 39, 45
NG = 53


class Op:
    __slots__ = ("q", "sem", "fn", "deps", "qidx", "semidx", "waits", "signal", "sigval", "clock", "inc")


class Prog:
    QUEUES = ("pe", "act", "dve", "pool", "sp")
    NDMA = 12

    def __init__(self):
        self.ops = []
        self.qops = {q: [] for q in self.QUEUES}
        self.semcount = {}
        self.lastw = {}
        self.readers = {}
        self.dma_rr = {}
        self.last_on_sem = {}

    def add(self, q, fn, reads=(), writes=(), dma=False):
        op = Op()
        op.q = q
        op.fn = fn
        op.signal = False
        op.waits = []
        deps = set()
        pk = [k for k in reads if isinstance(k, str) and k.startswith("ps")]
        if pk:
            reads = [k for k in reads if k not in pk]
            writes = list(writes) + pk
        for k in reads:
            w = self.lastw.get(k)
            if w is not None:
                deps.add(w)
        for k in writes:
            w = self.lastw.get(k)
            if w is not None:
                deps.add(w)
            for r in self.readers.get(k, ()):
                deps.add(r)
        for k in reads:
            self.readers.setdefault(k, []).append(op)
        for k in writes:
            self.lastw[k] = op
            self.readers[k] = []
        if dma:
            rr = self.dma_rr.get(q, 0)
            op.sem = "dma_%s_%d" % (q, rr % self.NDMA)
            self.dma_rr[q] = rr + 1
            op.inc = 16
            op.signal = True
            prev = self.last_on_sem.get(op.sem)
            if prev is not None:
                deps.add(prev)
        else:
            op.sem = q
            op.inc = 1
        self.last_on_sem[op.sem] = op
        deps.discard(op)
        op.deps = deps
        op.qidx = len(self.qops[q])
        op.semidx = self.semcount.get(op.sem, 0)
        self.semcount[op.sem] = op.semidx + 1
        self.qops[q].append(op)
        self.ops.append(op)
        return op

    def fence(self, exclude=()):
        for _ in range(2):
            lasts = [o for o in self.last_on_sem.values() if o not in exclude]
            for q in self.QUEUES:
                op = self.add(q, None, (), ())
                for l in lasts:
                    if l is not op:
                        op.deps.add(l)

    def finalize(self):
        clocks = {q: {} for q in self.QUEUES}
        for op in self.ops:
            clk = clocks[op.q]
            need = {}
            for d in op.deps:
                if d.sem == op.q:
                    if op.q == "pe":
                        continue
                    if op.qidx - d.qidx >= 3:
                        continue
                if clk.get(d.sem, -1) >= d.semidx:
                    continue
                cur = need.get(d.sem)
                if cur is None or cur.semidx < d.semidx:
                    need[d.sem] = d
            if need:
                clk = dict(clk)
                for d in need.values():
                    d.signal = True
                    for s, v in d.clock.items():
                        if clk.get(s, -1) < v:
                            clk[s] = v
                    if clk.get(d.sem, -1) < d.semidx:
                        clk[d.sem] = d.semidx
                clocks[op.q] = clk
                op.waits = list(need.values())
            op.clock = clk
        cnt = {}
        for op in self.ops:
            if op.signal:
                cnt[op.sem] = cnt.get(op.sem, 0) + op.inc
                op.sigval = cnt[op.sem]

    def replay(self, q, e, sems):
        for op in self.qops[q]:
            for d in op.waits:
                e.wait_ge(sems[d.sem], d.sigval)
            if op.fn is None:
                if op.signal:
                    e.nop(nofuse=True).then_inc(sems[op.sem], op.inc)
                continue
            ins = op.fn(e)
            if op.signal:
                ins.then_inc(sems[op.sem], op.inc)


class PsumPool:
    def __init__(self, n):
        self.free = list(range(n))

    def alloc(self):
        assert self.free, "psum exhausted"
        return self.free.pop(0)

    def release(self, b):
        self.free.append(b)


class Arena:
    BASE = 16512
    END = 229376

    def __init__(self, nc):
        self.nc = nc
        self.cur = self.BASE
        self.n = 0
        self.off = {}

    def alloc(self, name, shape, dt, at=None):
        nbytes = int(np.prod(shape[1:])) * (2 if dt == BF16 else 4)
        nbytes = (nbytes + 63) // 64 * 64
        if at is None:
            off = self.cur
            self.cur += nbytes
        else:
            off = at
        assert off + nbytes <= self.END, ("sbuf overflow", name, off, nbytes)
        self.n += 1
        self.off[name] = off
        return self.nc.alloc_sbuf_tensor_at("%s_%d" % (name, self.n), list(shape), dt, offset=off)


def build_program(cfg=None):
    cfg = cfg or {}
    n_seq = cfg.get("n_seq", SEQ_PER_CORE)
    n_layers = cfg.get("n_layers", 2)
    do_mixer = cfg.get("mixer", True)
    do_ffn = cfg.get("ffn", True)
    n_units_cap = cfg.get("n_units", 99)

    nc = bass.Bass("TRN2", target_bir_lowering=False)
    for fn, why in ((getattr(nc, "allow_low_precision", None), "bf16 matmul operands, fp32 accumulation"),
                    (getattr(nc, "allow_non_contiguous_dma", None), "weight re-layout")):
        if fn is not None:
            try:
                fn(why)
            except Exception:
                pass
    P = Prog()
    PS = PsumPool(8)

    def din(name, shape):
        return nc.dram_tensor(name, list(shape), F32, kind="ExternalInput").ap()

    x_d = din("x", [SEQ_PER_CORE, S, D])
    mem_d = din("mem", [SEQ_PER_CORE, MEMT, D])
    win_d = din("w_in_ext", [2, D, WIN_COLS])
    wq_d = din("w_q_ext", [2, 256, WQ_COLS])
    wkv_d = din("w_kv_up", [2, 128, 1024])
    wmem_d = din("w_mem_kv", [2, D, 512])
    wout_d = din("w_out", [2, D, D])
    wdgu_d = din("w_dense_gu", [1, D, 5632])
    wdd_d = din("w_dense_down", [1, 2816, D])
    wr_d = din("w_router", [1, D, 8])
    wegu_d = din("w_expert_gu", [1, 8, D, 2816])
    wed_d = din("w_expert_down", [1, 8, EFF, D])
    gains_d = din("gains", [2, 128, NG])
    y_d = nc.dram_tensor("y", [SEQ_PER_CORE, S, D], F32, kind="ExternalOutput").ap()
    xs_d = nc.dram_tensor("xs_scratch", [SEQ_PER_CORE, KC, 128, S], F32).ap()

    ps = [nc.alloc_psum_tensor("psb%d" % i, [128, 512], F32) for i in range(8)]
    NDUMP = 24
    dump_on = bool(cfg.get("dump"))
    dbg_d = nc.dram_tensor("dbg", [NDUMP, 128, 4096], F32, kind="ExternalOutput").ap() if dump_on else None
    ntb_cap = cfg.get("ntb", NTB)

    def PK(b):
        return "ps%d" % b

    A = Arena(nc)
    ident = A.alloc("ident", [128, 128], F32)
    onesf = A.alloc("onesf", [128, 64], F32)
    epsc = A.alloc("epsc", [128, 8], F32)
    ones1024 = A.alloc("ones1024", [128, 128], BF16)
    ones512 = A.alloc("ones512", [128, 128], BF16)
    ones256 = A.alloc("ones256", [128, 128], BF16)
    ones128 = A.alloc("ones128", [128, 128], BF16)
    blk96 = A.alloc("blk96", [128, 128], BF16)
    blk64 = A.alloc("blk64", [128, 128], BF16)
    cmask = A.alloc("cmask", [128, 128], BF16)
    selT = A.alloc("selT", [8, 8, 128], F32)
    pidx = A.alloc("pidx", [128, 8], F32)
    colidx = A.alloc("colidx", [128, 128], F32)
    gains = A.alloc("gains", [128, 2, NG], F32)
    COS = A.alloc("COS", [128, S], BF16)
    SINS = A.alloc("SINS", [128, S], BF16)
    PERS_END = A.cur

    w_in = A.alloc("w_in", [128, KC, WIN_COLS], BF16)
    w_q = A.alloc("w_q", [128, 2, WQ_COLS], BF16)
    w_kv = A.alloc("w_kv", [128, 1024], BF16)
    wo_mla = A.alloc("wo_mla", [64, 8, D], BF16)
    wo_mem = A.alloc("wo_mem", [64, 4, D], BF16)
    wo_conv = A.alloc("wo_conv", [128, 2, D], BF16)
    kfT = A.alloc("kfT", [96, NH, S], BF16)
    Vaug = A.alloc("Vaug", [128, S // 128, NH, 65], BF16)
    kmT = A.alloc("kmT", [128, 2, MEMT], BF16)
    vmaug = A.alloc("vmaug", [128, 2, NMH, 65], BF16)
    xt = A.alloc("xt", [128, KC, TB], F32)
    stage = A.alloc("stage", [128, 4, D], F32)
    R1 = A.off["stage"]
    SQ = A.alloc("SQ", [128, KC, TB], BF16, at=R1)
    hT = A.alloc("hT", [128, KC, TB], BF16, at=R1 + 8192)
    qf = A.alloc("qf", [96, NH, TB], BF16, at=R1)
    omla = A.alloc("omla", [64, NH, TB], BF16, at=R1 + 8192)
    qlat = A.alloc("qlat", [128, 2, TB], F32)
    R2 = A.off["qlat"]
    kvlat = A.alloc("kvlat", [128, TB], F32)
    t1 = A.alloc("t1", [128, TB], F32, at=R2)
    t2 = A.alloc("t2", [128, TB], F32, at=R2 + 2048)
    r96 = A.alloc("r96", [128, TB], F32, at=R2 + 4096)
    u_sb = A.alloc("u_sb", [128, TB], F32)
    R3 = A.off["u_sb"]
    ybuf = A.alloc("ybuf", [128, TB], F32)
    oconv = A.alloc("oconv", [128, 2, TB], F32)
    R3_END = A.cur
    PT = [A.alloc("PT%d" % i, [128, TB], BF16, at=R3 + i * 1024) for i in range(4)]
    drow = A.alloc("drow", [128, TB], F32, at=R3 + 4096)
    rec = A.alloc("rec", [128, TB], F32, at=R3 + 6144)
    rec2 = A.alloc("rec2", [128, TB], F32)
    assert R3 + 8192 <= R3_END
    vbuf = A.alloc("vbuf", [128, 2, TB + 16], F32)
    omem = A.alloc("omem", [64, NMH, TB], BF16)
    sq2 = A.alloc("sq2", [128, 2, TB], BF16)
    qlatn = A.alloc("qlatn", [128, 2, TB], BF16)
    kvlatn = A.alloc("kvlatn", [128, TB], BF16)
    rstd = [A.alloc("rstd%d" % i, [128, TB], F32) for i in range(2)]
    sq96 = A.alloc("sq96", [128, TB], BF16)
    sq96b = A.alloc("sq96b", [128, TB], BF16)
    t1b = A.alloc("t1b", [128, TB], F32)
    t2b = A.alloc("t2b", [128, TB], F32)
    r96b = A.alloc("r96b", [128, TB], F32)
    qm = A.alloc("qm", [128, 2, TB], BF16)
    ocn = A.alloc("ocn", [128, 2, TB], BF16)
    MIX_END = A.cur
    XT0 = A.off["xt"]
    memtok = A.alloc("memtok", [128, 2, D], F32, at=XT0)
    wmem = A.alloc("wmem", [128, KC, 512], BF16, at=XT0 + 8192)
    memT = A.alloc("memT", [128, KC, MEMT], BF16, at=XT0 + 16384)
    msq = A.alloc("msq", [128, D], BF16, at=XT0 + 20480)
    mss = A.alloc("mss", [128, 8], F32, at=XT0 + 22528)
    KF0 = A.off["kfT"]
    itmp = [A.alloc("itmp%d" % i, [128, S], F32, at=KF0 + i * 8192) for i in range(3)]
    iint = A.alloc("iint", [128, S], mybir.dt.int32, at=KF0 + 3 * 8192)
    pint = A.alloc("pint", [128, 8], mybir.dt.int32, at=KF0 + 4 * 8192)

    A.cur = PERS_END
    HT = A.alloc("HT", [128, KC, S], BF16)
    NGU, NDW = 4, 3
    gu = [A.alloc("gu%d" % i, [128, KC, 256], BF16) for i in range(NGU)]
    assert A.cur >= A.off["wo_mla"] + 16384, "prefetched mixer weights must lie inside HT+gu"
    XF = A.alloc("XF", [128, KC, S], F32)
    ABUF = A.alloc("ABUF", [128, NJ, S], BF16)
    AB0 = A.off["ABUF"]
    FSQ = A.alloc("FSQ", [128, KC, TB], BF16, at=AB0)
    ostage = A.alloc("ostage", [128, D], F32, at=AB0 + 8192)
    CW = A.alloc("CW", [128, S], F32)
    tmpb = [A.alloc("tmpb%d" % i, [128, TB], F32) for i in range(2)]
    ssb = [A.alloc("ssb%d" % i, [128, TB], F32) for i in range(2)]
    dw = [A.alloc("dw%d" % i, [128, NJ, 128], BF16) for i in range(NDW)]
    frstd = A.alloc("frstd", [128, TB], F32)
    combT = A.alloc("combT", [40, S], F32)
    wr_sb = A.alloc("wr_sb", [128, KC, 8], F32)
    wrg = A.alloc("wrg", [128, KC, 8], F32)
    rt = A.alloc("rt", [128, 64], F32)
    rt2 = A.alloc("rt2", [128, 64], F32)
    rt3 = A.alloc("rt3", [128, 64], F32)
    FFN_END = A.cur
    if cfg.get("verbose"):
        print("SBUF: pers_end", PERS_END, "mix_end", MIX_END, "ffn_end", FFN_END, "limit", Arena.END)

    def MM(out, lhsT, rhs, start, stop, r, w):
        P.add("pe", lambda e: e.matmul(out, lhsT, rhs, start=start, stop=stop), r, w)

    def TR(out, in_, r, w):
        P.add("pe", lambda e: e.transpose(out, in_, ident[:]), r, w)

    def ACT(out, in_, func, r, w, scale=1.0, bias=0.0, accum=None):
        if accum is None:
            P.add("act", lambda e: e.activation(out, in_, func, bias=bias, scale=scale), r, w)
        else:
            P.add("act", lambda e: e.activation(out, in_, func, bias=bias, scale=scale, accum_out=accum), r, w)

    def TS(out, in0, s1, s2, op0, op1, r, w, q="dve"):
        if op1 is None:
            P.add(q, lambda e: e.tensor_scalar(out, in0, s1, None, op0), r, w)
        else:
            P.add(q, lambda e: e.tensor_scalar(out, in0, s1, s2, op0, op1), r, w)

    def STT(out, in0, scalar, in1, op0, op1, r, w, q="dve"):
        P.add(q, lambda e: e.scalar_tensor_tensor(out, in0, scalar, in1, op0, op1), r, w)

    def TT(out, in0, in1, op, r, w, q="dve"):
        P.add(q, lambda e: e.tensor_tensor(out, in0, in1, op), r, w)

    def MEMSET(ap, val, w, q="dve"):
        P.add(q, lambda e: e.memset(ap, val), (), w)

    def DMA(q, out, in_, r, w):
        return P.add(q, lambda e: e.dma_start(out=out, in_=in_), r, w, dma=True)

    def DUMP(i, ap, keys):
        if not dump_on:
            return
        shp = ap.shape
        if len(shp) == 2:
            dst = dbg_d[i, 0:shp[0], 0:shp[1]]
        else:
            dst = dbg_d[i, 0:shp[0], 0:shp[1] * shp[2]].rearrange("p (a b) -> p a b", a=shp[1])
        DMA("pool", dst, ap, keys, [("dbg", i)])

    def RSTD(out, in_ps, r, w):
        p0 = out.base_partition()
        if cfg.get("arsqrt"):
            ACT(out, in_ps, AF.Abs_reciprocal_sqrt, r, w, bias=epsc[p0:p0 + out.shape[0], 0:1])
            return
        ACT(out, in_ps, AF.Ln, r, w, bias=epsc[p0:p0 + out.shape[0], 0:1])
        ACT(out, out, AF.Exp, w, w, scale=-0.5)

    def gcol(l, c, p0=0, p1=128):
        return gains[p0:p1, l, c:c + 1]

    def emit_init():
        P.add("pool", lambda e: e.iota(pidx[:, 0:1], [[0, 1]], base=0, channel_multiplier=1,
                                       allow_small_or_imprecise_dtypes=True), (), ["pidx"])
        P.add("pool", lambda e: e.iota(colidx[:], [[1, 128]], base=0, channel_multiplier=0,
                                       allow_small_or_imprecise_dtypes=True), (), ["colidx"])
        P.add("pool", lambda e: e.iota(itmp[0][:], [[1, S]], base=0, channel_multiplier=0,
                                       allow_small_or_imprecise_dtypes=True), (), ["itmp0"])
        P.add("pool", lambda e: e.iota(selT[:], [[1, 8], [0, 128]], base=0, channel_multiplier=0,
                                       allow_small_or_imprecise_dtypes=True), (), ["selT"])
        DMA("sp", gains[:], gains_d.rearrange("l p g -> p l g"), (), ["gains"])
        TS(ident[:], colidx[:], pidx[:, 0:1], None, ALU.is_equal, None, ["colidx", "pidx"], ["ident"])
        TS(cmask[:], colidx[:], pidx[:, 0:1], None, ALU.is_ge, None, ["colidx", "pidx"], ["cmask"])
        TS(selT[:], selT[:], pidx[0:8, 0:1], None, ALU.is_equal, None, ["selT", "pidx"], ["selT"])
        MEMSET(onesf[:], 1.0, ["onesf"])
        MEMSET(epsc[:], EPS, ["epsc"])
        MEMSET(ones1024[:], 1.0 / 1024, ["ones1024"])
        MEMSET(ones512[:], 1.0 / 512, ["ones512"])
        MEMSET(ones256[:], 1.0 / 256, ["ones256"])
        MEMSET(ones128[:], 1.0 / 128, ["ones128"])
        MEMSET(blk96[:], 0.0, ["blk96"])
        MEMSET(blk96[0:64, 0:64], 1.0 / 64, ["blk96"])
        MEMSET(blk96[64:96, 64:96], 1.0 / 32, ["blk96"])
        MEMSET(blk64[:], 0.0, ["blk64"])
        MEMSET(blk64[0:64, 0:64], 1.0 / 64, ["blk64"])
        MEMSET(blk64[64:128, 64:128], 1.0 / 64, ["blk64"])
        P.add("pool", lambda e: e.iota(pint[:, 0:1], [[0, 1]], base=0, channel_multiplier=1), (), ["pint"])
        P.add("dve", lambda e: e.tensor_single_scalar(pint[:, 1:2], pint[:, 0:1], 15, ALU.bitwise_and), ["pint"], ["pint"])
        P.add("dve", lambda e: e.tensor_single_scalar(pint[:, 2:3], pint[:, 0:1], 16, ALU.bitwise_and), ["pint"], ["pint"])
        P.add("dve", lambda e: e.tensor_copy(pidx[:, 1:2], pint[:, 1:2]), ["pint"], ["pidx"])
        P.add("dve", lambda e: e.tensor_copy(pidx[:, 3:4], pint[:, 2:3]), ["pint"], ["pidx"])
        ACT(pidx[:, 2:3], pidx[:, 1:2], AF.Exp, ["pidx"], ["pidx"], scale=-math.log(10000.0) / 16.0)
        TS(pidx[:, 4:5], pidx[:, 3:4], -1.0 / 8.0, 1.0, ALU.mult, ALU.add, ["pidx"], ["pidx"])
        two_pi = 2.0 * math.pi

        def reduce_sin(dst, shift, post_scalar_ap, post_imm):
            TS(itmp[1][:], itmp[0][:], pidx[:, 2:3], shift, ALU.mult, ALU.add, ["itmp0", "pidx"], ["itmp1"])
            TS(itmp[2][:], itmp[1][:], 1.0 / two_pi, None, ALU.mult, None, ["itmp1"], ["itmp2"])
            P.add("dve", lambda e: e.tensor_copy(iint[:], itmp[2][:]), ["itmp2"], ["iint"])
            P.add("dve", lambda e: e.tensor_copy(itmp[2][:], iint[:]), ["iint"], ["itmp2"])
            STT(itmp[1][:], itmp[2][:], -two_pi, itmp[1][:], ALU.mult, ALU.add, ["itmp2", "itmp1"], ["itmp1"])
            TS(itmp[2][:], itmp[1][:], math.pi, -two_pi, ALU.is_gt, ALU.mult, ["itmp1"], ["itmp2"])
            TT(itmp[1][:], itmp[1][:], itmp[2][:], ALU.add, ["itmp1", "itmp2"], ["itmp1"])
            TS(itmp[2][:], itmp[1][:], -math.pi, two_pi, ALU.is_lt, ALU.mult, ["itmp1"], ["itmp2"])
            TT(itmp[1][:], itmp[1][:], itmp[2][:], ALU.add, ["itmp1", "itmp2"], ["itmp1"])
            ACT(itmp[1][:], itmp[1][:], AF.Sin, ["itmp1"], ["itmp1"])
            if post_scalar_ap is not None:
                TS(dst, itmp[1][:], post_scalar_ap, post_imm, ALU.mult, ALU.mult, ["itmp1", "pidx"], ["tab"])
            else:
                P.add("dve", lambda e: e.tensor_copy(dst, itmp[1][:]), ["itmp1"], ["tab"])

        reduce_sin(SINS[:], 0.0, pidx[:, 4:5], -1.0)
        reduce_sin(COS[:], math.pi / 2, None, None)

    HG_KEYS = [("HT", t_) for t_ in range(NTB)] + [("gu", i_) for i_ in range(4)]

    def emit_mixer_weights_main(l, prefetch=False):
        ex = HG_KEYS if prefetch else []
        DMA("pool", w_in[:], win_d[l].rearrange("(k p) n -> p k n", p=128), (), ["w_in"] + ex)
        DMA("pool", w_q[:], wq_d[l].rearrange("(k p) n -> p k n", p=128), (), ["w_q"] + ex)
        DMA("pool", w_kv[:], wkv_d[l], (), ["w_kv"] + ex)
        DMA("pool", wo_mla[:], wout_d[l, 0:512, :].rearrange("(h p) n -> p h n", p=64), (), ["wo"] + ex)

    def emit_mixer_weights(l, have_main):
        if not have_main:
            emit_mixer_weights_main(l)
        DMA("pool", wo_mem[:], wout_d[l, 512:768, :].rearrange("(h p) n -> p h n", p=64), (), ["wo"])
        DMA("pool", wo_conv[:], wout_d[l, 768:1024, :].rearrange("(k p) n -> p k n", p=128), (), ["wo"])
        DMA("pool", wmem[:], wmem_d[l].rearrange("(k p) n -> p k n", p=128), (), ["wmem"])

    def emit_mem_path(s, l):
        MEMSET(Vaug[:, :, :, 64:65], 1.0, ["Vaug"])
        MEMSET(vmaug[:, :, :, 64:65], 1.0, ["vmaug"])
        DMA("sp", memtok[:], mem_d[s].rearrange("(j p) f -> p j f", p=128), (), ["memtok"])
        MEMSET(mss[:], 0.0, ["mss"])
        for j in range(2):
            ACT(msq[:], memtok[:, j, :], AF.Square, ["memtok"], ["msq", "mss"], accum=mss[:, j:j + 1])
        ACT(mss[:, 2:4], mss[:, 0:2], AF.Ln, ["mss"], ["mss"], scale=1.0 / D, bias=epsc[:, 0:1])
        ACT(mss[:, 4:6], mss[:, 2:4], AF.Exp, ["mss"], ["mss"], scale=-0.5)
        for j in range(2):
            ACT(memtok[:, j, :], memtok[:, j, :], AF.Copy, ["memtok", "mss"], ["memtok"], scale=mss[:, 4 + j:5 + j])
        for cp in range(4):
            b = PS.alloc()
            for ci in range(2):
                c = cp * 2 + ci
                for j in range(2):
                    TR(ps[b][:, ci * 256 + j * 128: ci * 256 + (j + 1) * 128], memtok[:, j, c * 128:(c + 1) * 128],
                       ["memtok", "ident"], [PK(b)])
            for ci in range(2):
                c = cp * 2 + ci
                TS(memT[:, c, :], ps[b][:, ci * 256:(ci + 1) * 256], gcol(l, G_MEM + c), None, ALU.mult, None,
                   [PK(b), "gains"], ["memT"])
            PS.release(b)
        for cc in range(2):
            b = PS.alloc()
            for k in range(KC):
                MM(ps[b][:, 0:MEMT], wmem[:, k, cc * 128:(cc + 1) * 128], memT[:, k, :], k == 0, k == KC - 1,
                   ["wmem", "memT"], [PK(b)])
            ACT(sq96[:, 0:MEMT], ps[b][:, 0:MEMT], AF.Square, [PK(b)], ["sq96"])
            b2 = PS.alloc()
            MM(ps[b2][:, 0:MEMT], blk64[:], sq96[:, 0:MEMT], True, True, ["blk64", "sq96"], [PK(b2)])
            RSTD(rstd[0][:, 0:MEMT], ps[b2][:, 0:MEMT], [PK(b2)], ["rstd0"])
            PS.release(b2)
            STT(kmT[:, cc, :], ps[b][:, 0:MEMT], gcol(l, G_KMEM), rstd[0][:, 0:MEMT], ALU.mult, ALU.mult,
                [PK(b), "rstd0", "gains"], ["kmT"])
            PS.release(b)
        for j in range(2):
            b = PS.alloc()
            for k in range(KC):
                MM(ps[b][:, 0:256], memT[:, k, j * 128:(j + 1) * 128], wmem[:, k, 256:512], k == 0, k == KC - 1,
                   ["wmem", "memT"], [PK(b)])
            ACT(vmaug[:, j, :, 0:64], ps[b][:, 0:256].rearrange("p (h d) -> p h d", h=NMH), AF.Copy,
                [PK(b)], ["vmaug"])
            PS.release(b)

    def emit_load_xt(s, l, t):
        c0 = t * TB
        if l == 0:
            DMA("sp", stage[:], x_d[s, c0:c0 + TB, :].rearrange("(j p) f -> p j f", p=128), (), ["R1", "R1b"])
            for c in range(KC):
                b = PS.alloc()
                for j in range(4):
                    TR(ps[b][:, j * 128:(j + 1) * 128], stage[:, j, c * 128:(c + 1) * 128], ["R1", "R1b", "ident"], [PK(b)])
                if c % 2 == 0:
                    ACT(xt[:, c, :], ps[b][:], AF.Copy, [PK(b)], [("xt", c)])
                else:
                    P.add("dve", lambda e, o=xt[:, c, :], i=ps[b][:]: e.tensor_copy(o, i), [PK(b)], [("xt", c)])
                PS.release(b)
        else:
            for c in range(KC):
                DMA("sp", xt[:, c, :], xs_d[s, c, :, c0:c0 + TB], (), [("xt", c)])

    XTK = [("xt", c) for c in range(KC)]

    def emit_m1(s, l, t):
        c0 = t * TB
        ACT(SQ[:], xt[:], AF.Square, XTK, ["R1"])
        b = PS.alloc()
        for k in range(KC):
            MM(ps[b][:], ones1024[:], SQ[:, k, :], k == 0, k == KC - 1, ["R1", "ones1024"], [PK(b)])
        if t == 0 and dump_on:
            P.add("dve", lambda e, o=t2[:], i_=ps[b][:]: e.tensor_copy(o, i_), [PK(b)], ["R2"])
            DUMP(17, t2[:], ["R2"])
        RSTD(rstd[0][:], ps[b][:], [PK(b)], ["rstd0"])
        PS.release(b)
        if t == 0:
            DUMP(14, SQ[:], ["R1"])
            DUMP(15, rstd[0][:], ["rstd0"])
            DUMP(16, xt[:], XTK)
        for k in range(KC):
            STT(hT[:, k, :], xt[:, k, :], gcol(l, G_MIX + k), rstd[0][:], ALU.mult, ALU.mult,
                [("xt", k), "rstd0", "gains"], [("hT", k), "R1b"])
        HTK = [("hT", k) for k in range(KC)]
        if t == 0:
            DUMP(0, hT[:], HTK)

        def inproj(col0, M):
            bb = PS.alloc()
            for k in range(KC):
                MM(ps[bb][0:M, :], w_in[:, k, col0:col0 + M], hT[:, k, :], k == 0, k == KC - 1,
                   ["w_in", ("hT", k), "R1b"], [PK(bb)])
            return bb

        for k2 in range(2):
            bb = inproj(k2 * 128, 128)
            ACT(qlat[:, k2, :], ps[bb][:], AF.Copy, [PK(bb)], ["R2"])
            ACT(sq2[:, k2, :], ps[bb][:], AF.Square, [PK(bb)], ["sq2"])
            PS.release(bb)
        b = PS.alloc()
        for k2 in range(2):
            MM(ps[b][:], ones256[:], sq2[:, k2, :], k2 == 0, k2 == 1, ["sq2", "ones256"], [PK(b)])
        RSTD(rstd[1][:], ps[b][:], [PK(b)], ["rstd1"])
        PS.release(b)
        for k2 in range(2):
            STT(qlatn[:, k2, :], qlat[:, k2, :], gcol(l, G_QLAT + k2), rstd[1][:], ALU.mult, ALU.mult,
                ["R2", "rstd1", "gains"], ["qlatn"])
        if t == 0:
            DUMP(1, qlatn[:], ["qlatn"])
        bb = inproj(256, 128)
        ACT(kvlat[:], ps[bb][:], AF.Copy, [PK(bb)], ["R2"])
        ACT(sq2[:, 0, :], ps[bb][:], AF.Square, [PK(bb)], ["sq2"])
        PS.release(bb)
        b = PS.alloc()
        MM(ps[b][:], ones128[:], sq2[:, 0, :], True, True, ["sq2", "ones128"], [PK(b)])
        RSTD(rstd[0][:], ps[b][:], [PK(b)], ["rstd0"])
        PS.release(b)
        STT(kvlatn[:], kvlat[:], gcol(l, G_KVLAT), rstd[0][:], ALU.mult, ALU.mult, ["R2", "rstd0", "gains"], ["kvlatn"])
        if t == 0:
            DUMP(2, kvlatn[:], ["kvlatn"])
        bA = inproj(320, 96)
        bB = inproj(1376, 96)
        kS, kR, kT1, kT2 = ("sq96", 1), ("r96", 1), ("t1", 1), ("t2", 1)
        ACT(sq96b[0:96, :], ps[bA][0:96, :], AF.Square, [PK(bA)], [kS])
        b = PS.alloc()
        MM(ps[b][0:96, :], blk96[0:96, 0:96], sq96b[0:96, :], True, True, [kS, "blk96"], [PK(b)])
        RSTD(r96b[64:96, :], ps[b][64:96, :], [PK(b)], [kR])
        PS.release(b)
        STT(t1b[64:96, :], ps[bA][64:96, :], gcol(l, G_K96, 64, 96), COS[64:96, c0:c0 + TB], ALU.mult, ALU.mult,
            [PK(bA), "COS", "gains"], [kT1])
        STT(t2b[64:96, :], ps[bB][64:96, :], gcol(l, G_K96SW, 64, 96), SINS[64:96, c0:c0 + TB], ALU.mult, ALU.mult,
            [PK(bB), "SINS", "gains"], [kT2])
        PS.release(bA)
        PS.release(bB)
        TT(t1b[64:96, :], t1b[64:96, :], t2b[64:96, :], ALU.add, [kT1, kT2], [kT1])
        TT(kfT[64:96, :, c0:c0 + TB], t1b[64:96, :].unsqueeze(1).to_broadcast([32, NH, TB]),
           r96b[64:96, :].unsqueeze(1).to_broadcast([32, NH, TB]), ALU.mult, [kT1, kR], [("kpe", t)])
        for cc in range(2):
            bb = inproj(416 + cc * 128, 128)
            ACT(sq2[:, cc, :], ps[bb][:], AF.Square, [PK(bb)], ["sq2"])
            b = PS.alloc()
            MM(ps[b][:], blk64[:], sq2[:, cc, :], True, True, ["sq2", "blk64"], [PK(b)])
            RSTD(rstd[cc][:], ps[b][:], [PK(b)], ["rstd%d" % cc])
            PS.release(b)
            STT(qm[:, cc, :], ps[bb][:], gcol(l, G_QMEM), rstd[cc][:], ALU.mult, ALU.mult,
                [PK(bb), "rstd%d" % cc, "gains"], ["qm"])
            PS.release(bb)
        if t == 0:
            MEMSET(vbuf[:, :, 0:2], 0.0, ["R3"])
        else:
            P.add("dve", lambda e: e.tensor_copy(vbuf[:, :, 0:2], vbuf[:, :, TB:TB + 2]), ["R3"], ["R3"])
        for cc in range(2):
            bgb = inproj(672 + cc * 128, 128)
            bgc = inproj(928 + cc * 128, 128)
            bu = inproj(1184 + cc * 128, 128)
            ACT(u_sb[:], ps[bu][:], AF.Copy, [PK(bu)], ["R3"])
            PS.release(bu)
            TT(vbuf[:, cc, 2:TB + 2], ps[bgc][:], u_sb[:], ALU.mult, [PK(bgc), "R3"], ["R3"])
            PS.release(bgc)
            ACT(ybuf[:], vbuf[:, cc, 2:TB + 2], AF.Copy, ["R3", "gains"], ["R3"], scale=gcol(l, G_CONVW + 2 * 2 + cc))
            STT(ybuf[:], vbuf[:, cc, 1:TB + 1], gcol(l, G_CONVW + 1 * 2 + cc), ybuf[:], ALU.mult, ALU.add,
                ["R3", "gains"], ["R3"])
            STT(ybuf[:], vbuf[:, cc, 0:TB], gcol(l, G_CONVW + 0 * 2 + cc), ybuf[:], ALU.mult, ALU.add,
                ["R3", "gains"], ["R3"])
            TT(oconv[:, cc, :], ps[bgb][:], ybuf[:], ALU.mult, [PK(bgb), "R3"], ["R3"])
            PS.release(bgb)
        ACT(sq2[:], oconv[:], AF.Square, ["R3"], ["sq2"])
        b = PS.alloc()
        for cc in range(2):
            MM(ps[b][:], ones256[:], sq2[:, cc, :], cc == 0, cc == 1, ["sq2", "ones256"], [PK(b)])
        RSTD(rstd[0][:], ps[b][:], [PK(b)], ["rstd0"])
        PS.release(b)
        for cc in range(2):
            STT(ocn[:, cc, :], oconv[:, cc, :], gcol(l, G_OCONV + cc), rstd[0][:], ALU.mult, ALU.mult,
                ["R3", "rstd0", "gains"], ["ocn"])
        wkv_v = w_kv[:, :].rearrange("p (h c) -> p h c", h=NH)[:, :, 64:128]
        for j in range(4):
            b = PS.alloc()
            MM(ps[b][:].rearrange("p (h d) -> p h d", h=NH), kvlatn[:, j * 128:(j + 1) * 128], wkv_v, True, True,
               ["w_kv", "kvlatn"], [PK(b)])
            ACT(Vaug[:, t * 4 + j, :, 0:64], ps[b][:].rearrange("p (h d) -> p h d", h=NH), AF.Copy,
                [PK(b)], [("Vaug", t)])
            PS.release(b)

        T1 = (t1, t1b)
        T2 = (t2, t2b)
        R96 = (r96, r96b)
        SQ96 = (sq96, sq96b)

        def prep_stages(h):
            pz = h % 2
            kT1, kT2, kR, kS = ("t1", pz), ("t2", pz), ("r96", pz), ("sq96", pz)
            bA = PS.alloc()
            bB = PS.alloc()
            bK = PS.alloc()
            for k2 in range(2):
                MM(ps[bA][0:96, :], w_q[:, k2, h * 96:(h + 1) * 96], qlatn[:, k2, :], k2 == 0, k2 == 1,
                   ["w_q", "qlatn"], [PK(bA)])
            for k2 in range(2):
                MM(ps[bB][0:96, :], w_q[:, k2, 768 + h * 96:768 + (h + 1) * 96], qlatn[:, k2, :], k2 == 0, k2 == 1,
                   ["w_q", "qlatn"], [PK(bB)])
            MM(ps[bK][0:64, :], w_kv[:, h * 128:h * 128 + 64], kvlatn[:], True, True, ["w_kv", "kvlatn"], [PK(bK)])
            yield
            ACT(SQ96[pz][0:96, :], ps[bA][0:96, :], AF.Square, [PK(bA)], [kS])
            ACT(sq2[0:64, pz, :], ps[bK][0:64, :], AF.Square, [PK(bK)], [("sq2", pz)])
            yield
            b = PS.alloc()
            MM(ps[b][0:96, :], blk96[0:96, 0:96], SQ96[pz][0:96, :], True, True, [kS, "blk96"], [PK(b)])
            yield
            RSTD(R96[pz][0:96, :], ps[b][0:96, :], [PK(b), "R2"], [kR])
            PS.release(b)
            yield
            b2 = PS.alloc()
            MM(ps[b2][0:64, :], blk96[0:64, 0:64], sq2[0:64, pz, :], True, True, [("sq2", pz), "blk96"], [PK(b2)])
            STT(qf[0:64, h, :], ps[bA][0:64, :], gcol(l, G_Q96, 0, 64), R96[pz][0:64, :], ALU.mult, ALU.mult,
                [PK(bA), kR, "gains", "R1"], [("qf", h)])
            STT(T1[pz][64:96, :], ps[bA][64:96, :], gcol(l, G_Q96, 64, 96), COS[64:96, c0:c0 + TB], ALU.mult, ALU.mult,
                [PK(bA), "COS", "gains", "R2"], [kT1])
            PS.release(bA)
            yield
            RSTD(rstd[pz][0:64, :], ps[b2][0:64, :], [PK(b2)], ["rstd%d" % pz])
            PS.release(b2)
            STT(T2[pz][64:96, :], ps[bB][64:96, :], gcol(l, G_Q96SW, 64, 96), SINS[64:96, c0:c0 + TB], ALU.mult, ALU.mult,
                [PK(bB), "SINS", "gains", "R2"], [kT2])
            PS.release(bB)
            yield
            TT(T1[pz][64:96, :], T1[pz][64:96, :], T2[pz][64:96, :], ALU.add, [kT1, kT2], [kT1])
            TT(qf[64:96, h, :], T1[pz][64:96, :], R96[pz][64:96, :], ALU.mult, [kT1, kR, "R1"], [("qf", h)])
            STT(kfT[0:64, h, c0:c0 + TB], ps[bK][0:64, :], gcol(l, G_K96, 0, 64), rstd[pz][0:64, :],
                ALU.mult, ALU.mult, [PK(bK), "rstd%d" % pz, "gains"], [("kfT", t, h)])
            PS.release(bK)

        NSTAGE = 7

        sc_mla = 1.0 / math.sqrt(96.0)
        sc_mem = 1.0 / math.sqrt(64.0)
        VR = [("Vaug", tt) for tt in range(t + 1)]
        LA = cfg.get("LA", 2)
        pending = []
        deferred = []
        acc = {}
        ctr = [0]

        def tick():
            for dd in list(deferred):
                dd[0] -= 1
                if dd[0] <= 0:
                    deferred.remove(dd)
                    dd[1]()

        def front(blk):
            kind, h, kb, nkb = blk
            i = ctr[0]
            ctr[0] += 1
            bS = PS.alloc()
            pt = PT[i % 4]
            if kind == "mla":
                jl = kb - 4 * t
                q0 = jl * 128 if jl > 0 else 0
                MM(ps[bS][:, q0:TB], kfT[0:96, h, kb * 128:(kb + 1) * 128], qf[0:96, h, q0:TB], True, True,
                   [("kfT", kb // 4, h), ("kpe", kb // 4), ("qf", h), "R1"], [PK(bS)])
                ACT(pt[:, q0:TB], ps[bS][:, q0:TB], AF.Exp, [PK(bS)], [("PT", i % 4)], scale=sc_mla)
                if jl >= 0:
                    TT(pt[:, q0:q0 + 128], pt[:, q0:q0 + 128], cmask[:], ALU.mult, [("PT", i % 4), "cmask"],
                       [("PT", i % 4)])
            else:
                q0 = 0
                cc, r0 = h // 2, (h % 2) * 64
                MM(ps[bS][:, :], kmT[r0:r0 + 64, cc, kb * 128:(kb + 1) * 128], qm[r0:r0 + 64, cc, :], True, True,
                   ["kmT", "qm"], [PK(bS)])
                ACT(pt[:, :], ps[bS][:, :], AF.Exp, [PK(bS)], [("PT", i % 4)], scale=sc_mem)
            PS.release(bS)
            pending.append((blk, i, q0))

        def back():
            (kind, h, kb, nkb), i, q0 = pending.pop(0)
            pt = PT[i % 4]
            if kb == 0:
                acc[(kind, h)] = PS.alloc()
            bO = acc[(kind, h)]
            if kind == "mla":
                MM(ps[bO][0:65, q0:TB], Vaug[:, kb, h, :], pt[:, q0:TB], kb == 0, kb == nkb - 1,
                   VR + [("PT", i % 4)], [PK(bO)])
            else:
                MM(ps[bO][0:65, :], vmaug[:, kb, h, :], pt[:, :], kb == 0, kb == nkb - 1,
                   ["vmaug", ("PT", i % 4)], [PK(bO)])
            if kb == nkb - 1:
                del acc[(kind, h)]
                dk = ("drow", h % 2)
                dr = drow if h % 2 == 0 else rec2
                ACT(dr[64:65, :], ps[bO][64:65, :], AF.Copy, [PK(bO)], [dk])

                def fin(kind=kind, h=h, bO=bO, dr=dr, dk=dk):
                    bD = PS.alloc()
                    MM(ps[bD][0:64, :], onesf[64:65, 0:64], dr[64:65, :], True, True, [dk, "onesf"], [PK(bD)])
                    dst, dkey = (omla, "R1b") if kind == "mla" else (omem, "omem")
                    P.add("dve", lambda e, o=rec[0:64, :], i_=ps[bD][0:64, :]: e.reciprocal(o, i_), [PK(bD)], ["rec"])
                    PS.release(bD)
                    TT(dst[0:64, h, :], ps[bO][0:64, :], rec[0:64, :], ALU.mult, [PK(bO), "rec"], [dkey])
                    PS.release(bO)

                deferred.append([2, fin])

        def push(blk):
            front(blk)
            tick()
            if len(pending) > LA:
                back()

        def attn_head(h, gen):
            nkb = 4 * (t + 1)
            done = 0
            for kb in range(nkb):
                push(("mla", h, kb, nkb))
                if gen is not None:
                    span = max(1, int(nkb * cfg.get("prep_span", 0.01)))
                    want = min(NSTAGE, ((kb + 1) * NSTAGE + span - 1) // span)
                    while done < want:
                        if next(gen, "END") == "END":
                            done = NSTAGE
                            break
                        done += 1
            if gen is not None:
                for _ in gen:
                    pass

        if (t == 0 and cfg.get("prep_first", 0)) or cfg.get("prep_first_all", 0):
            nxt = 0
            active = []
            while nxt < NH or active:
                if nxt < NH and (not active or (len(active) < 2 and active[0][1] >= 3)):
                    active.append([prep_stages(nxt), 0])
                    nxt += 1
                for a_ in list(active):
                    if next(a_[0], "END") == "END":
                        active.remove(a_)
                    else:
                        a_[1] += 1
            for h in range(NH):
                attn_head(h, None)
        else:
            g0 = prep_stages(0)
            for _ in g0:
                pass
            for h in range(NH):
                attn_head(h, prep_stages(h + 1) if h + 1 < NH else None)
        for h in range(NMH):
            for kb in range(2):
                push(("mem", h, kb, 2))
        while pending:
            back()
            tick()
        while deferred:
            tick()
        if t == 0:
            DUMP(3, qm[:], ["qm"])
            DUMP(4, ocn[:], ["ocn"])
            DUMP(5, qf[0:96, :, :], [("qf", h) for h in range(NH)])
            DUMP(6, kfT[0:96, :, 0:TB], [("kfT", 0, h) for h in range(NH)] + [("kpe", 0)])
            DUMP(7, Vaug[:, 0:4, :, :].rearrange("p a h d -> p a (h d)"), [("Vaug", 0)])
            DUMP(8, kmT[:], ["kmT"])
            DUMP(9, vmaug[:].rearrange("p a h d -> p a (h d)"), ["vmaug"])

    def emit_attention(s, l, t):
        return

    def emit_outproj(s, l, t, last_layer_no_ffn=False):
        c0 = t * TB
        if t == 0:
            DUMP(10, omla[0:64, :, :], ["R1b"])
            DUMP(11, omem[0:64, :, :], ["omem"])
        for (buf, key, nh, ones_m, gbase) in ((omla, "R1b", NH, ones512, G_OMLA), (omem, "omem", NMH, ones256, G_OMEM)):
            ACT(SQ[0:64, 0:nh, :], buf[0:64, :, :], AF.Square, [key], ["R1"])
            b = PS.alloc()
            for h in range(nh):
                MM(ps[b][0:64, :], ones_m[0:64, 0:64], SQ[0:64, h, :], h == 0, h == nh - 1, ["R1"], [PK(b)])
            RSTD(rstd[0][0:64, :], ps[b][0:64, :], [PK(b)], ["rstd0"])
            PS.release(b)
            for h in range(nh):
                STT(buf[0:64, h, :], buf[0:64, h, :], gcol(l, gbase + h, 0, 64), rstd[0][0:64, :], ALU.mult, ALU.mult,
                    [key, "rstd0", "gains"], [key])
        if t == 0:
            DUMP(12, omla[0:64, :, :], ["R1b"])
            DUMP(13, omem[0:64, :, :], ["omem"])
        for m in range(KC):
            b = PS.alloc()
            nmm = NH + NMH + 2
            i = 0
            for h in range(NH):
                MM(ps[b][:], wo_mla[0:64, h, m * 128:(m + 1) * 128], omla[0:64, h, :], i == 0, i == nmm - 1,
                   ["wo", "R1b"], [PK(b)])
                i += 1
            for h in range(NMH):
                MM(ps[b][:], wo_mem[0:64, h, m * 128:(m + 1) * 128], omem[0:64, h, :], i == 0, i == nmm - 1,
                   ["wo", "omem"], [PK(b)])
                i += 1
            for cc in range(2):
                MM(ps[b][:], wo_conv[:, cc, m * 128:(m + 1) * 128], ocn[:, cc, :], i == 0, i == nmm - 1,
                   ["wo", "ocn"], [PK(b)])
                i += 1
            TT(xt[:, m, :], xt[:, m, :], ps[b][:], ALU.add, [("xt", m), PK(b)], [("xt", m)])
            PS.release(b)
            DMA("sp", xs_d[s, m, :, c0:c0 + TB], xt[:, m, :], [("xt", m)], [("xs", s)])


    def emit_ffn(s, l, final, prefetch_l=None):
        for t in range(NTB):
            for c in range(KC):
                DMA("sp", XF[:, c, t * TB:(t + 1) * TB], xs_d[s, c, :, t * TB:(t + 1) * TB], [("xs", s)], [("XFl", c, t)])
        XFK = [("XF", c) for c in range(KC)]
        moe = (l % 2 == 1)
        skip = (n_units_cap == 0)
        if moe and not skip:
            DMA("sp", wr_sb[:], wr_d[0].rearrange("(k p) e -> p k e", p=128), (), ["wr_sb"])
            for k in range(KC):
                TS(wrg[:, k, :], wr_sb[:, k, :], gcol(l, G_FFN + k), None, ALU.mult, None, ["wr_sb", "gains"], ["wrg"])
        for t in range(0 if not skip else NTB, NTB):
            c0 = t * TB
            ACT(FSQ[:], XF[:, :, c0:c0 + TB], AF.Square, XFK + [("XFl", c, t) for c in range(KC)], ["FSQ"])
            b = PS.alloc()
            for k in range(KC):
                MM(ps[b][:], ones1024[:], FSQ[:, k, :], k == 0, k == KC - 1, ["FSQ", "ones1024"], [PK(b)])
            RSTD(frstd[:], ps[b][:], [PK(b)], ["frstd"])
            PS.release(b)
            for k in range(KC):
                STT(HT[:, k, c0:c0 + TB], XF[:, k, c0:c0 + TB], gcol(l, G_FFN + k), frstd[:], ALU.mult, ALU.mult,
                    [("XF", k), ("XFl", k, t), "frstd", "gains"], [("HT", t)])
            if moe:
                ACT(combT[32:33, c0:c0 + TB], frstd[32:33, :], AF.Copy, ["frstd"], ["rsrow"])

        rt_state = {"bT": None, "pend": []}
        RT = (rt, rt2, rt3)

        def router_transpose(jb):
            t_, j = jb // 4, jb % 4
            r_ = RT[jb % 3]
            rk = ("rt", jb % 3)
            if j == 0:
                rt_state["bT"] = PS.alloc()
            bT = rt_state["bT"]
            TR(ps[bT][0:8, j * 128:(j + 1) * 128], r_[:, 48:56], [rk, "ident"], [PK(bT)])
            if j == 3:
                ACT(combT[0:8, t_ * TB:(t_ + 1) * TB], ps[bT][0:8, :], AF.Copy, [PK(bT)], ["combT"])
                PS.release(bT)

        def router_block(jb):
            if len(rt_state["pend"]) >= 2:
                router_transpose(rt_state["pend"].pop(0))
            r_ = RT[jb % 3]
            rk = ("rt", jb % 3)
            tok = slice(jb * 128, (jb + 1) * 128)
            b = PS.alloc()
            for k in range(KC):
                MM(ps[b][:, 0:8], XF[:, k, tok], wrg[:, k, :], k == 0, k == KC - 1, [("XF", k), "wrg"], [PK(b)])
            MM(ps[b][:, 8:9], combT[32:33, tok], onesf[32:33, 0:1], True, True, ["rsrow", "onesf"], [PK(b)])
            ACT(r_[:, 8:9], ps[b][:, 8:9], AF.Copy, [PK(b)], [rk])
            TS(r_[:, 0:8], ps[b][:, 0:8], r_[:, 8:9], None, ALU.mult, None, [PK(b), rk], [rk])
            PS.release(b)
            P.add("dve", lambda e: e.max(r_[:, 16:24], r_[:, 0:8]), [rk], [rk])
            TS(r_[:, 24:32], r_[:, 0:8], r_[:, 16:17], None, ALU.subtract, None, [rk], [rk])
            ACT(r_[:, 24:32], r_[:, 24:32], AF.Exp, [rk], [rk])
            TS(r_[:, 32:40], r_[:, 0:8], r_[:, 17:18], None, ALU.is_ge, None, [rk], [rk])
            TT(r_[:, 24:32], r_[:, 24:32], r_[:, 32:40], ALU.mult, [rk], [rk])
            P.add("dve", lambda e: e.reduce_sum(r_[:, 40:41], r_[:, 24:32], AX.X), [rk], [rk])
            P.add("dve", lambda e: e.reciprocal(r_[:, 41:42], r_[:, 40:41]), [rk], [rk])
            TS(r_[:, 48:56], r_[:, 24:32], r_[:, 41:42], None, ALU.mult, None, [rk], [rk])
            rt_state["pend"].append(jb)

        def router_flush():
            while rt_state["pend"]:
                router_transpose(rt_state["pend"].pop(0))

        router_todo = list(range(S // 128)) if (moe and not skip) else []
        if not cfg.get("router_overlap", 1):
            while router_todo:
                router_block(router_todo.pop(0))
            router_flush()
        if not moe:
            li = l // 2
            units = [(wdgu_d[li], 0, 2816, wdd_d[li], 0, None), (wdgu_d[li], EFF, 2816 + EFF, wdd_d[li], EFF, None)]
        else:
            li = l // 2
            units = [(wegu_d[li, e], 0, EFF, wed_d[li, e], 0, e) for e in range(8)]
        units = units[:n_units_cap]
        gu_i = [0]
        dw_i = [0]
        HTK = [("HT", t) for t in range(NTB)]
        for ui, (wgu, gc0, uc0, wd, dr0, ex) in enumerate(units):
            def emit_cw(ex=ex):
                for t in range(NTB):
                    b = PS.alloc()
                    MM(ps[b][:], selT[0:8, ex, :], combT[0:8, t * TB:(t + 1) * TB], True, True, ["selT", "combT"], [PK(b)])
                    ACT(CW[:, t * TB:(t + 1) * TB], ps[b][:], AF.Copy, [PK(b)], ["CW"])
                    PS.release(b)

            cw_late = ex is not None and bool(router_todo)
            if ex is not None and not cw_late:
                emit_cw()
            for j in range(NJ):
                sl = gu_i[0] % NGU
                gu_i[0] += 1
                DMA("pool", gu[sl][:, :, 0:128],
                    wgu[:, gc0 + j * 128: gc0 + (j + 1) * 128].rearrange("(k p) n -> p k n", p=128), (), [("gu", sl)])
                DMA("pool", gu[sl][:, :, 128:256],
                    wgu[:, uc0 + j * 128: uc0 + (j + 1) * 128].rearrange("(k p) n -> p k n", p=128), (), [("gu", sl)])
                for t in range(NTB):
                    bg = PS.alloc()
                    bu = PS.alloc()
                    for k in range(KC):
                        MM(ps[bg][:], gu[sl][:, k, 0:128], HT[:, k, t * TB:(t + 1) * TB], k == 0, k == KC - 1,
                           [("gu", sl), ("HT", t)], [PK(bg)])
                    for k in range(KC):
                        MM(ps[bu][:], gu[sl][:, k, 128:256], HT[:, k, t * TB:(t + 1) * TB], k == 0, k == KC - 1,
                           [("gu", sl), ("HT", t)], [PK(bu)])
                    si = (j * NTB + t) % 2
                    ACT(ssb[si][:], ps[bg][:], AF.Silu, [PK(bg)], [("ssb", si)])
                    PS.release(bg)
                    TT(ABUF[:, j, t * TB:(t + 1) * TB], ssb[si][:], ps[bu][:], ALU.mult, [("ssb", si), PK(bu)],
                       [("A", j, t)])
                    PS.release(bu)
                for _ in range(2):
                    if router_todo:
                        router_block(router_todo.pop(0))
            while router_todo:
                router_block(router_todo.pop(0))
            router_flush()
            if cw_late:
                emit_cw()
            for m in range(KC):
                if m == NDW and prefetch_l is not None and ui == len(units) - 1:
                    emit_mixer_weights_main(prefetch_l, prefetch=True)
                sl = dw_i[0] % NDW
                dw_i[0] += 1
                DMA("pool", dw[sl][:], wd[dr0:dr0 + EFF, m * 128:(m + 1) * 128].rearrange("(j p) n -> p j n", p=128),
                    (), [("dw", sl)])
                for t in range(NTB):
                    b = PS.alloc()
                    for j in range(NJ):
                        MM(ps[b][:], dw[sl][:, j, :], ABUF[:, j, t * TB:(t + 1) * TB], j == 0, j == NJ - 1,
                           [("dw", sl), ("A", j, t)], [PK(b)])
                    xsl = XF[:, m, t * TB:(t + 1) * TB]
                    if ex is not None:
                        ti = (m * NTB + t) % 2
                        TT(tmpb[ti][:], ps[b][:], CW[:, t * TB:(t + 1) * TB], ALU.mult, [PK(b), "CW"], [("tmpb", ti)])
                        TT(xsl, xsl, tmpb[ti][:], ALU.add, [("XF", m), ("tmpb", ti)], [("XF", m)])
                    else:
                        TT(xsl, xsl, ps[b][:], ALU.add, [("XF", m), PK(b)], [("XF", m)])
                    PS.release(b)
        if final:
            outs = []
            for tk in range(S // 128):
                for half in range(2):
                    b = PS.alloc()
                    for ci in range(4):
                        c = half * 4 + ci
                        TR(ps[b][:, ci * 128:(ci + 1) * 128], XF[:, c, tk * 128:(tk + 1) * 128], [("XF", c), "ident"],
                           [PK(b)])
                    if half == 0:
                        ACT(ostage[:, 0:512], ps[b][:], AF.Copy, [PK(b)] + [("A", j, t) for j in range(4) for t in range(NTB)],
                            ["ostage"])
                    else:
                        P.add("dve", lambda e, o=ostage[:, 512:1024], i_=ps[b][:]: e.tensor_copy(o, i_),
                              [PK(b)] + [("A", j, t) for j in range(4) for t in range(NTB)], ["ostage"])
                    PS.release(b)
                outs.append(DMA("sp", y_d[s, tk * 128:(tk + 1) * 128, :], ostage[:], ["ostage"], [("y", s)]))
            return outs
        else:
            for c in range(KC):
                DMA("sp", xs_d[s, c, :, :], XF[:, c, :], [("XF", c)], [("xs", s)])
            return []

    have_main = [False]
    early = set()
    if do_mixer and cfg.get("early_w", 1):
        n0 = len(P.ops)
        emit_mixer_weights_main(0)
        early = set(P.ops[n0:])
        have_main[0] = True
    emit_init()
    P.fence(exclude=early)
    out_dmas = []
    for s in range(n_seq):
        for l in range(n_layers):
            if do_mixer:
                emit_mixer_weights(l, have_main[0])
                have_main[0] = False
                emit_mem_path(s, l)
                P.fence()
                for t in range(ntb_cap):
                    emit_load_xt(s, l, t)
                    emit_m1(s, l, t)
                    emit_attention(s, l, t)
                    emit_outproj(s, l, t)
            else:
                for t in range(NTB):
                    emit_load_xt(s, l, t)
                    for m in range(KC):
                        DMA("sp", xs_d[s, m, :, t * TB:(t + 1) * TB], xt[:, m, :], [("xt", m)], [("xs", s)])
            P.fence()
            final = (l == n_layers - 1)
            is_last = (s == n_seq - 1 and l == n_layers - 1)
            pf = None if (is_last or not do_mixer or not cfg.get("prefetch", 1) or n_units_cap == 0) else (l + 1) % n_layers
            out_dmas += emit_ffn(s, l, final, pf)
            have_main[0] = pf is not None
            P.fence()
    fin = P.add("sp", None, (), ())
    for o in out_dmas:
        fin.deps.add(o)
    P.finalize()

    sem_names = list(P.semcount.keys())
    import contextlib
    with contextlib.ExitStack() as st:
        sems = {nm: st.enter_context(nc.semaphore("s_" + nm)) for nm in sem_names}
        block = st.enter_context(nc.Block())

        @block.tensor
        def _(e):
            P.replay("pe", e, sems)

        @block.scalar
        def _(e):
            P.replay("act", e, sems)

        @block.vector
        def _(e):
            P.replay("dve", e, sems)

        @block.gpsimd
        def _(e):
            P.replay("pool", e, sems)

        @block.sync
        def _(e):
            P.replay("sp", e, sems)

    return nc, P


def prep_weights(inp):
    f = lambda a: np.ascontiguousarray(np.asarray(a, dtype=np.float32))
    w_in = f(inp["w_in"])
    kpe = w_in[:, :, 384:416]
    w_in_ext = np.concatenate([w_in, kpe[:, :, 16:32], kpe[:, :, 0:16]], axis=2)
    wq = f(inp["w_q_up"]).reshape(2, 256, NH, 96)
    ext = np.concatenate([wq[..., 0:64], wq[..., 80:96], wq[..., 64:80]], axis=-1)
    w_q_ext = np.concatenate([wq.reshape(2, 256, 768), ext.reshape(2, 256, 768)], axis=2)
    G = np.zeros((2, 128, NG), np.float32)
    for l in range(2):
        def colk(v, n):
            return np.asarray(v, np.float32).reshape(n, 128).T
        G[l, :, G_MIX:G_MIX + 8] = colk(inp["g_mix"][l], 8)
        G[l, :, G_FFN:G_FFN + 8] = colk(inp["g_ffn"][l], 8)
        G[l, :, G_QLAT:G_QLAT + 2] = colk(inp["g_q_lat"][l], 2)
        G[l, :, G_KVLAT] = inp["g_kv_lat"][l]
        gq = np.asarray(inp["g_q_mla"][l], np.float32)
        gk = np.asarray(inp["g_k_mla"][l], np.float32)
        G[l, 0:96, G_Q96] = gq
        G[l, 64:96, G_Q96SW] = np.concatenate([gq[80:96], gq[64:80]])
        G[l, 0:96, G_K96] = gk
        G[l, 64:96, G_K96SW] = np.concatenate([gk[80:96], gk[64:80]])
        G[l, :, G_QMEM] = np.tile(np.asarray(inp["g_q_mem"][l], np.float32), 2)
        G[l, :, G_KMEM] = np.tile(np.asarray(inp["g_k_mem"][l], np.float32), 2)
        go = np.asarray(inp["g_out"][l], np.float32)
        G[l, 0:64, G_OMLA:G_OMLA + 8] = go[0:512].reshape(8, 64).T
        G[l, 0:64, G_OMEM:G_OMEM + 4] = go[512:768].reshape(4, 64).T
        G[l, :, G_OCONV:G_OCONV + 2] = go[768:1024].reshape(2, 128).T
        cw = np.asarray(inp["conv_w"][l], np.float32)
        for tap in range(3):
            G[l, :, G_CONVW + tap * 2:G_CONVW + tap * 2 + 2] = cw[tap].reshape(2, 128).T
        G[l, :, G_MEM:G_MEM + 8] = colk(inp["g_mem"][l], 8)
    return {
        "w_in_ext": np.ascontiguousarray(w_in_ext),
        "w_q_ext": np.ascontiguousarray(w_q_ext),
        "w_kv_up": f(inp["w_kv_up"]),
        "w_mem_kv": f(inp["w_mem_kv"]),
        "w_out": f(inp["w_out"]),
        "w_dense_gu": f(inp["w_dense_gu"]),
        "w_dense_down": f(inp["w_dense_down"]),
        "w_router": f(inp["w_router"]),
        "w_expert_gu": f(inp["w_expert_gu"]),
        "w_expert_down": f(inp["w_expert_down"]),
        "gains": G,
    }


_CACHE = {}


def kernel(**inputs):
    x = np.asarray(inputs["x"], np.float32)
    mem = np.asarray(inputs["mem"], np.float32)
    shared = prep_weights(inputs)
    if "nc" not in _CACHE:
        _CACHE["nc"] = build_program()[0]
    nc = _CACHE["nc"]
    in_maps = []
    for c in range(NCORES):
        m = dict(shared)
        m["x"] = np.ascontiguousarray(x[c * SEQ_PER_CORE:(c + 1) * SEQ_PER_CORE])
        m["mem"] = np.ascontiguousarray(mem[c * SEQ_PER_CORE:(c + 1) * SEQ_PER_CORE])
        in_maps.append(m)
    res = run_bass_kernel_spmd(nc, in_maps, core_ids=list(range(NCORES)))
    out = np.concatenate([np.asarray(r["y"], np.float32) for r in res.results], axis=0)
    return out
```

```python
import math
import numpy as np
import concourse.bass as bass
import concourse.mybir as mybir
from concourse.bass_utils import run_bass_kernel_spmd

F32 = mybir.dt.float32
BF16 = mybir.dt.bfloat16
ALU = mybir.AluOpType
AF = mybir.ActivationFunctionType
AX = mybir.AxisListType

NCORES = 8
SEQ_PER_CORE = 2
S = 2048
D = 1024
KC = 8
TB = 512
NTB = S // TB
MEMT = 256
EPS = 1e-6
NH = 8
NMH = 4
EFF = 1408
NJ = EFF // 128
WIN_COLS = 1472
WQ_COLS = 1536

G_MIX, G_FFN, G_QLAT, G_KVLAT, G_Q96, G_Q96SW, G_K96, G_K96SW, G_QMEM, G_KMEM = 0, 8, 16, 18, 19, 20, 21, 22, 23, 24
G_OMLA, G_OMEM, G_OCONV, G_CONVW, G_MEM = 25, 33, 37, 39, 45
NG = 53


class Op:
    __slots__ = ("q", "sem", "fn", "deps", "qidx", "semidx", "waits", "signal", "sigval", "clock", "inc")


class Prog:
    QUEUES = ("pe", "act", "dve", "pool", "sp")
    NDMA = 12

    def __init__(self):
        self.ops = []
        self.qops = {q: [] for q in self.QUEUES}
        self.semcount = {}
        self.lastw = {}
        self.readers = {}
        self.dma_rr = {}
        self.last_on_sem = {}

    def add(self, q, fn, reads=(), writes=(), dma=False):
        op = Op()
        op.q = q
        op.fn = fn
        op.signal = False
        op.waits = []
        deps = set()
        pk = [k for k in reads if isinstance(k, str) and k.startswith("ps")]
        if pk:
            reads = [k for k in reads if k not in pk]
            writes = list(writes) + pk
        for k in reads:
            w = self.lastw.get(k)
            if w is not None:
                deps.add(w)
        for k in writes:
            w = self.lastw.get(k)
            if w is not None:
                deps.add(w)
            for r in self.readers.get(k, ()):
                deps.add(r)
        for k in reads:
            self.readers.setdefault(k, []).append(op)
        for k in writes:
            self.lastw[k] = op
            self.readers[k] = []
        if dma:
            rr = self.dma_rr.get(q, 0)
            op.sem = "dma_%s_%d" % (q, rr % self.NDMA)
            self.dma_rr[q] = rr + 1
            op.inc = 16
            op.signal = True
            prev = self.last_on_sem.get(op.sem)
            if prev is not None:
                deps.add(prev)
        else:
            op.sem = q
            op.inc = 1
        self.last_on_sem[op.sem] = op
        deps.discard(op)
        op.deps = deps
        op.qidx = len(self.qops[q])
        op.semidx = self.semcount.get(op.sem, 0)
        self.semcount[op.sem] = op.semidx + 1
        self.qops[q].append(op)
        self.ops.append(op)
        return op

    def fence(self, exclude=()):
        for _ in range(2):
            lasts = [o for o in self.last_on_sem.values() if o not in exclude]
            for q in self.QUEUES:
                op = self.add(q, None, (), ())
                for l in lasts:
                    if l is not op:
                        op.deps.add(l)

    def finalize(self):
        clocks = {q: {} for q in self.QUEUES}
        for op in self.ops:
            clk = clocks[op.q]
            need = {}
            for d in op.deps:
                if d.sem == op.q:
                    if op.q == "pe":
                        continue
                    if op.qidx - d.qidx >= 3:
                        continue
                if clk.get(d.sem, -1) >= d.semidx:
                    continue
                cur = need.get(d.sem)
                if cur is None or cur.semidx < d.semidx:
                    need[d.sem] = d
            if need:
                clk = dict(clk)
                for d in need.values():
                    d.signal = True
                    for s, v in d.clock.items():
                        if clk.get(s, -1) < v:
                            clk[s] = v
                    if clk.get(d.sem, -1) < d.semidx:
                        clk[d.sem] = d.semidx
                clocks[op.q] = clk
                op.waits = list(need.values())
            op.clock = clk
        cnt = {}
        for op in self.ops:
            if op.signal:
                cnt[op.sem] = cnt.get(op.sem, 0) + op.inc
                op.sigval = cnt[op.sem]

    def replay(self, q, e, sems):
        for op in self.qops[q]:
            for d in op.waits:
                e.wait_ge(sems[d.sem], d.sigval)
            if op.fn is None:
                if op.signal:
                    e.nop(nofuse=True).then_inc(sems[op.sem], op.inc)
                continue
            ins = op.fn(e)
            if op.signal:
                ins.then_inc(sems[op.sem], op.inc)


class PsumPool:
    def __init__(self, n):
        self.free = list(range(n))

    def alloc(self):
        assert self.free, "psum exhausted"
        return self.free.pop(0)

    def release(self, b):
        self.free.append(b)


class Arena:
    BASE = 16512
    END = 229376

    def __init__(self, nc):
        self.nc = nc
        self.cur = self.BASE
        self.n = 0
        self.off = {}

    def alloc(self, name, shape, dt, at=None):
        nbytes = int(np.prod(shape[1:])) * (2 if dt == BF16 else 4)
        nbytes = (nbytes + 63) // 64 * 64
        if at is None:
            off = self.cur
            self.cur += nbytes
        else:
            off = at
        assert off + nbytes <= self.END, ("sbuf overflow", name, off, nbytes)
        self.n += 1
        self.off[name] = off
        return self.nc.alloc_sbuf_tensor_at("%s_%d" % (name, self.n), list(shape), dt, offset=off)


def build_program(cfg=None):
    cfg = cfg or {}
    n_seq = cfg.get("n_seq", SEQ_PER_CORE)
    n_layers = cfg.get("n_layers", 2)
    do_mixer = cfg.get("mixer", True)
    do_ffn = cfg.get("ffn", True)
    n_units_cap = cfg.get("n_units", 99)

    nc = bass.Bass("TRN2", target_bir_lowering=False)
    for fn, why in ((getattr(nc, "allow_low_precision", None), "bf16 matmul operands, fp32 accumulation"),
                    (getattr(nc, "allow_non_contiguous_dma", None), "weight re-layout")):
        if fn is not None:
            try:
                fn(why)
            except Exception:
                pass
    P = Prog()
    PS = PsumPool(8)

    def din(name, shape):
        return nc.dram_tensor(name, list(shape), F32, kind="ExternalInput").ap()

    x_d = din("x", [SEQ_PER_CORE, S, D])
    mem_d = din("mem", [SEQ_PER_CORE, MEMT, D])
    win_d = din("w_in_ext", [2, D, WIN_COLS])
    wq_d = din("w_q_ext", [2, 256, WQ_COLS])
    wkv_d = din("w_kv_up", [2, 128, 1024])
    wmem_d = din("w_mem_kv", [2, D, 512])
    wout_d = din("w_out", [2, D, D])
    wdgu_d = din("w_dense_gu", [1, D, 5632])
    wdd_d = din("w_dense_down", [1, 2816, D])
    wr_d = din("w_router", [1, D, 8])
    wegu_d = din("w_expert_gu", [1, 8, D, 2816])
    wed_d = din("w_expert_down", [1, 8, EFF, D])
    gains_d = din("gains", [2, 128, NG])
    y_d = nc.dram_tensor("y", [SEQ_PER_CORE, S, D], F32, kind="ExternalOutput").ap()
    xs_d = nc.dram_tensor("xs_scratch", [SEQ_PER_CORE, KC, 128, S], F32).ap()

    ps = [nc.alloc_psum_tensor("psb%d" % i, [128, 512], F32) for i in range(8)]
    NDUMP = 24
    dump_on = bool(cfg.get("dump"))
    dbg_d = nc.dram_tensor("dbg", [NDUMP, 128, 4096], F32, kind="ExternalOutput").ap() if dump_on else None
    ntb_cap = cfg.get("ntb", NTB)

    def PK(b):
        return "ps%d" % b

    A = Arena(nc)
    ident = A.alloc("ident", [128, 128], F32)
    onesf = A.alloc("onesf", [128, 64], F32)
    epsc = A.alloc("epsc", [128, 8], F32)
    ones1024 = A.alloc("ones1024", [128, 128], BF16)
    ones512 = A.alloc("ones512", [128, 128], BF16)
    ones256 = A.alloc("ones256", [128, 128], BF16)
    ones128 = A.alloc("ones128", [128, 128], BF16)
    blk96 = A.alloc("blk96", [128, 128], BF16)
    blk64 = A.alloc("blk64", [128, 128], BF16)
    cmask = A.alloc("cmask", [128, 128], BF16)
    selT = A.alloc("selT", [8, 8, 128], F32)
    pidx = A.alloc("pidx", [128, 8], F32)
    colidx = A.alloc("colidx", [128, 128], F32)
    gains = A.alloc("gains", [128, 2, NG], F32)
    COS = A.alloc("COS", [128, S], BF16)
    SINS = A.alloc("SINS", [128, S], BF16)
    PERS_END = A.cur

    w_in = A.alloc("w_in", [128, KC, WIN_COLS], BF16)
    w_q = A.alloc("w_q", [128, 2, WQ_COLS], BF16)
    w_kv = A.alloc("w_kv", [128, 1024], BF16)
    wo_mla = A.alloc("wo_mla", [64, 8, D], BF16)
    wo_mem = A.alloc("wo_mem", [64, 4, D], BF16)
    wo_conv = A.alloc("wo_conv", [128, 2, D], BF16)
    kfT = A.alloc("kfT", [96, NH, S], BF16)
    Vaug = A.alloc("Vaug", [128, S // 128, NH, 65], BF16)
    kmT = A.alloc("kmT", [128, 2, MEMT], BF16)
    vmaug = A.alloc("vmaug", [128, 2, NMH, 65], BF16)
    xt = A.alloc("xt", [128, KC, TB], F32)
    stage = A.alloc("stage", [128, 4, D], F32)
    R1 = A.off["stage"]
    SQ = A.alloc("SQ", [128, KC, TB], BF16, at=R1)
    hT = A.alloc("hT", [128, KC, TB], BF16, at=R1 + 8192)
    qf = A.alloc("qf", [96, NH, TB], BF16, at=R1)
    omla = A.alloc("omla", [64, NH, TB], BF16, at=R1 + 8192)
    qlat = A.alloc("qlat", [128, 2, TB], F32)
    R2 = A.off["qlat"]
    kvlat = A.alloc("kvlat", [128, TB], F32)
    t1 = A.alloc("t1", [128, TB], F32, at=R2)
    t2 = A.alloc("t2", [128, TB], F32, at=R2 + 2048)
    r96 = A.alloc("r96", [128, TB], F32, at=R2 + 4096)
    u_sb = A.alloc("u_sb", [128, TB], F32)
    R3 = A.off["u_sb"]
    ybuf = A.alloc("ybuf", [128, TB], F32)
    oconv = A.alloc("oconv", [128, 2, TB], F32)
    R3_END = A.cur
    PT = [A.alloc("PT%d" % i, [128, TB], BF16, at=R3 + i * 1024) for i in range(4)]
    drow = A.alloc("drow", [128, TB], F32, at=R3 + 4096)
    rec = A.alloc("rec", [128, TB], F32, at=R3 + 6144)
    rec2 = A.alloc("rec2", [128, TB], F32)
    assert R3 + 8192 <= R3_END
    vbuf = A.alloc("vbuf", [128, 2, TB + 16], F32)
    omem = A.alloc("omem", [64, NMH, TB], BF16)
    sq2 = A.alloc("sq2", [128, 2, TB], BF16)
    qlatn = A.alloc("qlatn", [128, 2, TB], BF16)
    kvlatn = A.alloc("kvlatn", [128, TB], BF16)
    rstd = [A.alloc("rstd%d" % i, [128, TB], F32) for i in range(2)]
    sq96 = A.alloc("sq96", [128, TB], BF16)
    sq96b = A.alloc("sq96b", [128, TB], BF16)
    t1b = A.alloc("t1b", [128, TB], F32)
    t2b = A.alloc("t2b", [128, TB], F32)
    r96b = A.alloc("r96b", [128, TB], F32)
    qm = A.alloc("qm", [128, 2, TB], BF16)
    ocn = A.alloc("ocn", [128, 2, TB], BF16)
    MIX_END = A.cur
    XT0 = A.off["xt"]
    memtok = A.alloc("memtok", [128, 2, D], F32, at=XT0)
    wmem = A.alloc("wmem", [128, KC, 512], BF16, at=XT0 + 8192)
    memT = A.alloc("memT", [128, KC, MEMT], BF16, at=XT0 + 16384)
    msq = A.alloc("msq", [128, D], BF16, at=XT0 + 20480)
    mss = A.alloc("mss", [128, 8], F32, at=XT0 + 22528)
    KF0 = A.off["kfT"]
    itmp = [A.alloc("itmp%d" % i, [128, S], F32, at=KF0 + i * 8192) for i in range(3)]
    iint = A.alloc("iint", [128, S], mybir.dt.int32, at=KF0 + 3 * 8192)
    pint = A.alloc("pint", [128, 8], mybir.dt.int32, at=KF0 + 4 * 8192)

    A.cur = PERS_END
    HT = A.alloc("HT", [128, KC, S], BF16)
    NGU, NDW = 4, 3
    gu = [A.alloc("gu%d" % i, [128, KC, 256], BF16) for i in range(NGU)]
    assert A.cur >= A.off["wo_mla"] + 16384, "prefetched mixer weights must lie inside HT+gu"
    XF = A.alloc("XF", [128, KC, S], F32)
    ABUF = A.alloc("ABUF", [128, NJ, S], BF16)
    AB0 = A.off["ABUF"]
    FSQ = A.alloc("FSQ", [128, KC, TB], BF16, at=AB0)
    ostage = A.alloc("ostage", [128, D], F32, at=AB0 + 8192)
    CW = A.alloc("CW", [128, S], F32)
    tmpb = [A.alloc("tmpb%d" % i, [128, TB], F32) for i in range(2)]
    ssb = [A.alloc("ssb%d" % i, [128, TB], F32) for i in range(2)]
    dw = [A.alloc("dw%d" % i, [128, NJ, 128], BF16) for i in range(NDW)]
    frstd = A.alloc("frstd", [128, TB], F32)
    combT = A.alloc("combT", [40, S], F32)
    wr_sb = A.alloc("wr_sb", [128, KC, 8], F32)
    wrg = A.alloc("wrg", [128, KC, 8], F32)
    rt = A.alloc("rt", [128, 64], F32)
    rt2 = A.alloc("rt2", [128, 64], F32)
    rt3 = A.alloc("rt3", [128, 64], F32)
    FFN_END = A.cur
    if cfg.get("verbose"):
        print("SBUF: pers_end", PERS_END, "mix_end", MIX_END, "ffn_end", FFN_END, "limit", Arena.END)

    def MM(out, lhsT, rhs, start, stop, r, w):
        P.add("pe", lambda e: e.matmul(out, lhsT, rhs, start=start, stop=stop), r, w)

    def TR(out, in_, r, w):
        P.add("pe", lambda e: e.transpose(out, in_, ident[:]), r, w)

    def ACT(out, in_, func, r, w, scale=1.0, bias=0.0, accum=None):
        if accum is None:
            P.add("act", lambda e: e.activation(out, in_, func, bias=bias, scale=scale), r, w)
        else:
            P.add("act", lambda e: e.activation(out, in_, func, bias=bias, scale=scale, accum_out=accum), r, w)

    def TS(out, in0, s1, s2, op0, op1, r, w, q="dve"):
        if op1 is None:
            P.add(q, lambda e: e.tensor_scalar(out, in0, s1, None, op0), r, w)
        else:
            P.add(q, lambda e: e.tensor_scalar(out, in0, s1, s2, op0, op1), r, w)

    def STT(out, in0, scalar, in1, op0, op1, r, w, q="dve"):
        P.add(q, lambda e: e.scalar_tensor_tensor(out, in0, scalar, in1, op0, op1), r, w)

    def TT(out, in0, in1, op, r, w, q="dve"):
        P.add(q, lambda e: e.tensor_tensor(out, in0, in1, op), r, w)

    def MEMSET(ap, val, w, q="dve"):
        P.add(q, lambda e: e.memset(ap, val), (), w)

    def DMA(q, out, in_, r, w):
        return P.add(q, lambda e: e.dma_start(out=out, in_=in_), r, w, dma=True)

    def DUMP(i, ap, keys):
        if not dump_on:
            return
        shp = ap.shape
        if len(shp) == 2:
            dst = dbg_d[i, 0:shp[0], 0:shp[1]]
        else:
            dst = dbg_d[i, 0:shp[0], 0:shp[1] * shp[2]].rearrange("p (a b) -> p a b", a=shp[1])
        DMA("pool", dst, ap, keys, [("dbg", i)])

    def WARM(n):
        if n <= 0:
            return
        bw = PS.alloc()
        for _ in range(n):
            MM(ps[bw][:], ones1024[:], COS[:, 0:TB], True, True, ["ones1024", "COS"], [PK(bw)])
        PS.release(bw)

    def RSTD(out, in_ps, r, w):
        p0 = out.base_partition()
        if cfg.get("arsqrt"):
            ACT(out, in_ps, AF.Abs_reciprocal_sqrt, r, w, bias=epsc[p0:p0 + out.shape[0], 0:1])
            return
        ACT(out, in_ps, AF.Ln, r, w, bias=epsc[p0:p0 + out.shape[0], 0:1])
        ACT(out, out, AF.Exp, w, w, scale=-0.5)

    def gcol(l, c, p0=0, p1=128):
        return gains[p0:p1, l, c:c + 1]

    def emit_init():
        P.add("pool", lambda e: e.iota(pidx[:, 0:1], [[0, 1]], base=0, channel_multiplier=1,
                                       allow_small_or_imprecise_dtypes=True), (), ["pidx"])
        P.add("pool", lambda e: e.iota(colidx[:], [[1, 128]], base=0, channel_multiplier=0,
                                       allow_small_or_imprecise_dtypes=True), (), ["colidx"])
        P.add("pool", lambda e: e.iota(itmp[0][:], [[1, S]], base=0, channel_multiplier=0,
                                       allow_small_or_imprecise_dtypes=True), (), ["itmp0"])
        P.add("pool", lambda e: e.iota(selT[:], [[1, 8], [0, 128]], base=0, channel_multiplier=0,
                                       allow_small_or_imprecise_dtypes=True), (), ["selT"])
        DMA("sp", gains[:], gains_d.rearrange("l p g -> p l g"), (), ["gains"])
        TS(ident[:], colidx[:], pidx[:, 0:1], None, ALU.is_equal, None, ["colidx", "pidx"], ["ident"])
        TS(cmask[:], colidx[:], pidx[:, 0:1], None, ALU.is_ge, None, ["colidx", "pidx"], ["cmask"])
        TS(selT[:], selT[:], pidx[0:8, 0:1], None, ALU.is_equal, None, ["selT", "pidx"], ["selT"])
        MEMSET(onesf[:], 1.0, ["onesf"])
        MEMSET(epsc[:], EPS, ["epsc"])
        MEMSET(ones1024[:], 1.0 / 1024, ["ones1024"])
        MEMSET(ones512[:], 1.0 / 512, ["ones512"])
        MEMSET(ones256[:], 1.0 / 256, ["ones256"])
        MEMSET(ones128[:], 1.0 / 128, ["ones128"])
        MEMSET(blk96[:], 0.0, ["blk96"])
        MEMSET(blk96[0:64, 0:64], 1.0 / 64, ["blk96"])
        MEMSET(blk96[64:96, 64:96], 1.0 / 32, ["blk96"])
        MEMSET(blk64[:], 0.0, ["blk64"])
        MEMSET(blk64[0:64, 0:64], 1.0 / 64, ["blk64"])
        MEMSET(blk64[64:128, 64:128], 1.0 / 64, ["blk64"])
        P.add("pool", lambda e: e.iota(pint[:, 0:1], [[0, 1]], base=0, channel_multiplier=1), (), ["pint"])
        P.add("dve", lambda e: e.tensor_single_scalar(pint[:, 1:2], pint[:, 0:1], 15, ALU.bitwise_and), ["pint"], ["pint"])
        P.add("dve", lambda e: e.tensor_single_scalar(pint[:, 2:3], pint[:, 0:1], 16, ALU.bitwise_and), ["pint"], ["pint"])
        P.add("dve", lambda e: e.tensor_copy(pidx[:, 1:2], pint[:, 1:2]), ["pint"], ["pidx"])
        P.add("dve", lambda e: e.tensor_copy(pidx[:, 3:4], pint[:, 2:3]), ["pint"], ["pidx"])
        ACT(pidx[:, 2:3], pidx[:, 1:2], AF.Exp, ["pidx"], ["pidx"], scale=-math.log(10000.0) / 16.0)
        TS(pidx[:, 4:5], pidx[:, 3:4], -1.0 / 8.0, 1.0, ALU.mult, ALU.add, ["pidx"], ["pidx"])
        two_pi = 2.0 * math.pi

        def reduce_sin(dst, shift, post_scalar_ap, post_imm):
            TS(itmp[1][:], itmp[0][:], pidx[:, 2:3], shift, ALU.mult, ALU.add, ["itmp0", "pidx"], ["itmp1"])
            TS(itmp[2][:], itmp[1][:], 1.0 / two_pi, None, ALU.mult, None, ["itmp1"], ["itmp2"])
            P.add("dve", lambda e: e.tensor_copy(iint[:], itmp[2][:]), ["itmp2"], ["iint"])
            P.add("dve", lambda e: e.tensor_copy(itmp[2][:], iint[:]), ["iint"], ["itmp2"])
            STT(itmp[1][:], itmp[2][:], -two_pi, itmp[1][:], ALU.mult, ALU.add, ["itmp2", "itmp1"], ["itmp1"])
            TS(itmp[2][:], itmp[1][:], math.pi, -two_pi, ALU.is_gt, ALU.mult, ["itmp1"], ["itmp2"])
            TT(itmp[1][:], itmp[1][:], itmp[2][:], ALU.add, ["itmp1", "itmp2"], ["itmp1"])
            TS(itmp[2][:], itmp[1][:], -math.pi, two_pi, ALU.is_lt, ALU.mult, ["itmp1"], ["itmp2"])
            TT(itmp[1][:], itmp[1][:], itmp[2][:], ALU.add, ["itmp1", "itmp2"], ["itmp1"])
            ACT(itmp[1][:], itmp[1][:], AF.Sin, ["itmp1"], ["itmp1"])
            if post_scalar_ap is not None:
                TS(dst, itmp[1][:], post_scalar_ap, post_imm, ALU.mult, ALU.mult, ["itmp1", "pidx"], ["tab"])
            else:
                P.add("dve", lambda e: e.tensor_copy(dst, itmp[1][:]), ["itmp1"], ["tab"])

        reduce_sin(SINS[:], 0.0, pidx[:, 4:5], -1.0)
        reduce_sin(COS[:], math.pi / 2, None, None)

    HG_KEYS = [("HT", t_) for t_ in range(NTB)] + [("gu", i_) for i_ in range(4)]

    def emit_mixer_weights_main(l, prefetch=False):
        ex = HG_KEYS if prefetch else []
        DMA("pool", w_in[:], win_d[l].rearrange("(k p) n -> p k n", p=128), (), ["w_in"] + ex)
        DMA("pool", w_q[:], wq_d[l].rearrange("(k p) n -> p k n", p=128), (), ["w_q"] + ex)
        DMA("pool", w_kv[:], wkv_d[l], (), ["w_kv"] + ex)
        DMA("pool", wo_mla[:], wout_d[l, 0:512, :].rearrange("(h p) n -> p h n", p=64), (), ["wo"] + ex)

    def emit_mixer_weights(l, have_main):
        if not have_main:
            emit_mixer_weights_main(l)
        DMA("pool", wo_mem[:], wout_d[l, 512:768, :].rearrange("(h p) n -> p h n", p=64), (), ["wo"])
        DMA("pool", wo_conv[:], wout_d[l, 768:1024, :].rearrange("(k p) n -> p k n", p=128), (), ["wo"])
        DMA("pool", wmem[:], wmem_d[l].rearrange("(k p) n -> p k n", p=128), (), ["wmem"])

    def emit_mem_path(s, l):
        MEMSET(Vaug[:, :, :, 64:65], 1.0, ["Vaug"])
        MEMSET(vmaug[:, :, :, 64:65], 1.0, ["vmaug"])
        DMA("sp", memtok[:], mem_d[s].rearrange("(j p) f -> p j f", p=128), (), ["memtok"])
        MEMSET(mss[:], 0.0, ["mss"])
        for j in range(2):
            ACT(msq[:], memtok[:, j, :], AF.Square, ["memtok"], ["msq", "mss"], accum=mss[:, j:j + 1])
        ACT(mss[:, 2:4], mss[:, 0:2], AF.Ln, ["mss"], ["mss"], scale=1.0 / D, bias=epsc[:, 0:1])
        ACT(mss[:, 4:6], mss[:, 2:4], AF.Exp, ["mss"], ["mss"], scale=-0.5)
        for j in range(2):
            ACT(memtok[:, j, :], memtok[:, j, :], AF.Copy, ["memtok", "mss"], ["memtok"], scale=mss[:, 4 + j:5 + j])
        for cp in range(4):
            b = PS.alloc()
            for ci in range(2):
                c = cp * 2 + ci
                for j in range(2):
                    TR(ps[b][:, ci * 256 + j * 128: ci * 256 + (j + 1) * 128], memtok[:, j, c * 128:(c + 1) * 128],
                       ["memtok", "ident"], [PK(b)])
            for ci in range(2):
                c = cp * 2 + ci
                TS(memT[:, c, :], ps[b][:, ci * 256:(ci + 1) * 256], gcol(l, G_MEM + c), None, ALU.mult, None,
                   [PK(b), "gains"], ["memT"])
            PS.release(b)
        for cc in range(2):
            b = PS.alloc()
            for k in range(KC):
                MM(ps[b][:, 0:MEMT], wmem[:, k, cc * 128:(cc + 1) * 128], memT[:, k, :], k == 0, k == KC - 1,
                   ["wmem", "memT"], [PK(b)])
            ACT(sq96[:, 0:MEMT], ps[b][:, 0:MEMT], AF.Square, [PK(b)], ["sq96"])
            b2 = PS.alloc()
            MM(ps[b2][:, 0:MEMT], blk64[:], sq96[:, 0:MEMT], True, True, ["blk64", "sq96"], [PK(b2)])
            RSTD(rstd[0][:, 0:MEMT], ps[b2][:, 0:MEMT], [PK(b2)], ["rstd0"])
            PS.release(b2)
            STT(kmT[:, cc, :], ps[b][:, 0:MEMT], gcol(l, G_KMEM), rstd[0][:, 0:MEMT], ALU.mult, ALU.mult,
                [PK(b), "rstd0", "gains"], ["kmT"])
            PS.release(b)
        for j in range(2):
            b = PS.alloc()
            for k in range(KC):
                MM(ps[b][:, 0:256], memT[:, k, j * 128:(j + 1) * 128], wmem[:, k, 256:512], k == 0, k == KC - 1,
                   ["wmem", "memT"], [PK(b)])
            ACT(vmaug[:, j, :, 0:64], ps[b][:, 0:256].rearrange("p (h d) -> p h d", h=NMH), AF.Copy,
                [PK(b)], ["vmaug"])
            PS.release(b)

    def emit_load_xt(s, l, t):
        c0 = t * TB
        if l == 0:
            DMA("sp", stage[:], x_d[s, c0:c0 + TB, :].rearrange("(j p) f -> p j f", p=128), (), ["R1", "R1b"])
            for c in range(KC):
                b = PS.alloc()
                for j in range(4):
                    TR(ps[b][:, j * 128:(j + 1) * 128], stage[:, j, c * 128:(c + 1) * 128], ["R1", "R1b", "ident"], [PK(b)])
                if c % 2 == 0:
                    ACT(xt[:, c, :], ps[b][:], AF.Copy, [PK(b)], [("xt", c)])
                else:
                    P.add("dve", lambda e, o=xt[:, c, :], i=ps[b][:]: e.tensor_copy(o, i), [PK(b)], [("xt", c)])
                PS.release(b)
        else:
            for c in range(KC):
                DMA("sp", xt[:, c, :], xs_d[s, c, :, c0:c0 + TB], (), [("xt", c)])

    XTK = [("xt", c) for c in range(KC)]

    def emit_m1(s, l, t):
        c0 = t * TB
        ACT(SQ[:], xt[:], AF.Square, XTK, ["R1"])
        b = PS.alloc()
        for k in range(KC):
            MM(ps[b][:], ones1024[:], SQ[:, k, :], k == 0, k == KC - 1, ["R1", "ones1024"], [PK(b)])
        if t == 0 and dump_on:
            P.add("dve", lambda e, o=t2[:], i_=ps[b][:]: e.tensor_copy(o, i_), [PK(b)], ["R2"])
            DUMP(17, t2[:], ["R2"])
        RSTD(rstd[0][:], ps[b][:], [PK(b)], ["rstd0"])
        PS.release(b)
        if t == 0:
            DUMP(14, SQ[:], ["R1"])
            DUMP(15, rstd[0][:], ["rstd0"])
            DUMP(16, xt[:], XTK)
        for k in range(KC):
            STT(hT[:, k, :], xt[:, k, :], gcol(l, G_MIX + k), rstd[0][:], ALU.mult, ALU.mult,
                [("xt", k), "rstd0", "gains"], [("hT", k), "R1b"])
        HTK = [("hT", k) for k in range(KC)]
        if t == 0:
            DUMP(0, hT[:], HTK)

        def inproj(col0, M):
            bb = PS.alloc()
            for k in range(KC):
                MM(ps[bb][0:M, :], w_in[:, k, col0:col0 + M], hT[:, k, :], k == 0, k == KC - 1,
                   ["w_in", ("hT", k), "R1b"], [PK(bb)])
            return bb

        WARM(cfg.get("warm_in", 32))
        for k2 in range(2):
            bb = inproj(k2 * 128, 128)
            ACT(qlat[:, k2, :], ps[bb][:], AF.Copy, [PK(bb)], ["R2"])
            ACT(sq2[:, k2, :], ps[bb][:], AF.Square, [PK(bb)], ["sq2"])
            PS.release(bb)
        b = PS.alloc()
        for k2 in range(2):
            MM(ps[b][:], ones256[:], sq2[:, k2, :], k2 == 0, k2 == 1, ["sq2", "ones256"], [PK(b)])
        RSTD(rstd[1][:], ps[b][:], [PK(b)], ["rstd1"])
        PS.release(b)
        for k2 in range(2):
            STT(qlatn[:, k2, :], qlat[:, k2, :], gcol(l, G_QLAT + k2), rstd[1][:], ALU.mult, ALU.mult,
                ["R2", "rstd1", "gains"], ["qlatn"])
        if t == 0:
            DUMP(1, qlatn[:], ["qlatn"])
        bb = inproj(256, 128)
        ACT(kvlat[:], ps[bb][:], AF.Copy, [PK(bb)], ["R2"])
        ACT(sq2[:, 0, :], ps[bb][:], AF.Square, [PK(bb)], ["sq2"])
        PS.release(bb)
        b = PS.alloc()
        MM(ps[b][:], ones128[:], sq2[:, 0, :], True, True, ["sq2", "ones128"], [PK(b)])
        RSTD(rstd[0][:], ps[b][:], [PK(b)], ["rstd0"])
        PS.release(b)
        STT(kvlatn[:], kvlat[:], gcol(l, G_KVLAT), rstd[0][:], ALU.mult, ALU.mult, ["R2", "rstd0", "gains"], ["kvlatn"])
        if t == 0:
            DUMP(2, kvlatn[:], ["kvlatn"])
        bA = inproj(320, 96)
        bB = inproj(1376, 96)
        kS, kR, kT1, kT2 = ("sq96", 1), ("r96", 1), ("t1", 1), ("t2", 1)
        ACT(sq96b[0:96, :], ps[bA][0:96, :], AF.Square, [PK(bA)], [kS])
        b = PS.alloc()
        MM(ps[b][0:96, :], blk96[0:96, 0:96], sq96b[0:96, :], True, True, [kS, "blk96"], [PK(b)])
        RSTD(r96b[64:96, :], ps[b][64:96, :], [PK(b)], [kR])
        PS.release(b)
        STT(t1b[64:96, :], ps[bA][64:96, :], gcol(l, G_K96, 64, 96), COS[64:96, c0:c0 + TB], ALU.mult, ALU.mult,
            [PK(bA), "COS", "gains"], [kT1])
        STT(t2b[64:96, :], ps[bB][64:96, :], gcol(l, G_K96SW, 64, 96), SINS[64:96, c0:c0 + TB], ALU.mult, ALU.mult,
            [PK(bB), "SINS", "gains"], [kT2])
        PS.release(bA)
        PS.release(bB)
        TT(t1b[64:96, :], t1b[64:96, :], t2b[64:96, :], ALU.add, [kT1, kT2], [kT1])
        TT(kfT[64:96, :, c0:c0 + TB], t1b[64:96, :].unsqueeze(1).to_broadcast([32, NH, TB]),
           r96b[64:96, :].unsqueeze(1).to_broadcast([32, NH, TB]), ALU.mult, [kT1, kR], [("kpe", t)])
        for cc in range(2):
            bb = inproj(416 + cc * 128, 128)
            ACT(sq2[:, cc, :], ps[bb][:], AF.Square, [PK(bb)], ["sq2"])
            b = PS.alloc()
            MM(ps[b][:], blk64[:], sq2[:, cc, :], True, True, ["sq2", "blk64"], [PK(b)])
            RSTD(rstd[cc][:], ps[b][:], [PK(b)], ["rstd%d" % cc])
            PS.release(b)
            STT(qm[:, cc, :], ps[bb][:], gcol(l, G_QMEM), rstd[cc][:], ALU.mult, ALU.mult,
                [PK(bb), "rstd%d" % cc, "gains"], ["qm"])
            PS.release(bb)
        if t == 0:
            MEMSET(vbuf[:, :, 0:2], 0.0, ["R3"])
        else:
            P.add("dve", lambda e: e.tensor_copy(vbuf[:, :, 0:2], vbuf[:, :, TB:TB + 2]), ["R3"], ["R3"])
        for cc in range(2):
            bgb = inproj(672 + cc * 128, 128)
            bgc = inproj(928 + cc * 128, 128)
            bu = inproj(1184 + cc * 128, 128)
            ACT(u_sb[:], ps[bu][:], AF.Copy, [PK(bu)], ["R3"])
            PS.release(bu)
            TT(vbuf[:, cc, 2:TB + 2], ps[bgc][:], u_sb[:], ALU.mult, [PK(bgc), "R3"], ["R3"])
            PS.release(bgc)
            ACT(ybuf[:], vbuf[:, cc, 2:TB + 2], AF.Copy, ["R3", "gains"], ["R3"], scale=gcol(l, G_CONVW + 2 * 2 + cc))
            STT(ybuf[:], vbuf[:, cc, 1:TB + 1], gcol(l, G_CONVW + 1 * 2 + cc), ybuf[:], ALU.mult, ALU.add,
                ["R3", "gains"], ["R3"])
            STT(ybuf[:], vbuf[:, cc, 0:TB], gcol(l, G_CONVW + 0 * 2 + cc), ybuf[:], ALU.mult, ALU.add,
                ["R3", "gains"], ["R3"])
            TT(oconv[:, cc, :], ps[bgb][:], ybuf[:], ALU.mult, [PK(bgb), "R3"], ["R3"])
            PS.release(bgb)
        ACT(sq2[:], oconv[:], AF.Square, ["R3"], ["sq2"])
        b = PS.alloc()
        for cc in range(2):
            MM(ps[b][:], ones256[:], sq2[:, cc, :], cc == 0, cc == 1, ["sq2", "ones256"], [PK(b)])
        RSTD(rstd[0][:], ps[b][:], [PK(b)], ["rstd0"])
        PS.release(b)
        for cc in range(2):
            STT(ocn[:, cc, :], oconv[:, cc, :], gcol(l, G_OCONV + cc), rstd[0][:], ALU.mult, ALU.mult,
                ["R3", "rstd0", "gains"], ["ocn"])
        wkv_v = w_kv[:, :].rearrange("p (h c) -> p h c", h=NH)[:, :, 64:128]
        for j in range(4):
            b = PS.alloc()
            MM(ps[b][:].rearrange("p (h d) -> p h d", h=NH), kvlatn[:, j * 128:(j + 1) * 128], wkv_v, True, True,
               ["w_kv", "kvlatn"], [PK(b)])
            ACT(Vaug[:, t * 4 + j, :, 0:64], ps[b][:].rearrange("p (h d) -> p h d", h=NH), AF.Copy,
                [PK(b)], [("Vaug", t)])
            PS.release(b)

        T1 = (t1, t1b)
        T2 = (t2, t2b)
        R96 = (r96, r96b)
        SQ96 = (sq96, sq96b)

        def prep_stages(h):
            pz = h % 2
            kT1, kT2, kR, kS = ("t1", pz), ("t2", pz), ("r96", pz), ("sq96", pz)
            bA = PS.alloc()
            bB = PS.alloc()
            bK = PS.alloc()
            for k2 in range(2):
                MM(ps[bA][0:96, :], w_q[:, k2, h * 96:(h + 1) * 96], qlatn[:, k2, :], k2 == 0, k2 == 1,
                   ["w_q", "qlatn"], [PK(bA)])
            for k2 in range(2):
                MM(ps[bB][0:96, :], w_q[:, k2, 768 + h * 96:768 + (h + 1) * 96], qlatn[:, k2, :], k2 == 0, k2 == 1,
                   ["w_q", "qlatn"], [PK(bB)])
            MM(ps[bK][0:64, :], w_kv[:, h * 128:h * 128 + 64], kvlatn[:], True, True, ["w_kv", "kvlatn"], [PK(bK)])
            yield
            ACT(SQ96[pz][0:96, :], ps[bA][0:96, :], AF.Square, [PK(bA)], [kS])
            ACT(sq2[0:64, pz, :], ps[bK][0:64, :], AF.Square, [PK(bK)], [("sq2", pz)])
            yield
            WARM(cfg.get("warm_stats", 0))
            b = PS.alloc()
            MM(ps[b][0:96, :], blk96[0:96, 0:96], SQ96[pz][0:96, :], True, True, [kS, "blk96"], [PK(b)])
            yield
            RSTD(R96[pz][0:96, :], ps[b][0:96, :], [PK(b), "R2"], [kR])
            PS.release(b)
            yield
            b2 = PS.alloc()
            MM(ps[b2][0:64, :], blk96[0:64, 0:64], sq2[0:64, pz, :], True, True, [("sq2", pz), "blk96"], [PK(b2)])
            STT(qf[0:64, h, :], ps[bA][0:64, :], gcol(l, G_Q96, 0, 64), R96[pz][0:64, :], ALU.mult, ALU.mult,
                [PK(bA), kR, "gains", "R1"], [("qf", h)])
            STT(T1[pz][64:96, :], ps[bA][64:96, :], gcol(l, G_Q96, 64, 96), COS[64:96, c0:c0 + TB], ALU.mult, ALU.mult,
                [PK(bA), "COS", "gains", "R2"], [kT1])
            PS.release(bA)
            yield
            RSTD(rstd[pz][0:64, :], ps[b2][0:64, :], [PK(b2)], ["rstd%d" % pz])
            PS.release(b2)
            STT(T2[pz][64:96, :], ps[bB][64:96, :], gcol(l, G_Q96SW, 64, 96), SINS[64:96, c0:c0 + TB], ALU.mult, ALU.mult,
                [PK(bB), "SINS", "gains", "R2"], [kT2])
            PS.release(bB)
            yield
            pq = "pool" if cfg.get("pool_rope", 0) else "dve"
            TT(T1[pz][64:96, :], T1[pz][64:96, :], T2[pz][64:96, :], ALU.add, [kT1, kT2], [kT1], q=pq)
            TT(qf[64:96, h, :], T1[pz][64:96, :], R96[pz][64:96, :], ALU.mult, [kT1, kR, "R1"], [("qf", h)], q=pq)
            STT(kfT[0:64, h, c0:c0 + TB], ps[bK][0:64, :], gcol(l, G_K96, 0, 64), rstd[pz][0:64, :],
                ALU.mult, ALU.mult, [PK(bK), "rstd%d" % pz, "gains"], [("kfT", t, h)])
            PS.release(bK)

        NSTAGE = 7

        sc_mla = 1.0 / math.sqrt(96.0)
        sc_mem = 1.0 / math.sqrt(64.0)
        VR = [("Vaug", tt) for tt in range(t + 1)]
        LA = cfg.get("LA", 2)
        pending = []
        deferred = []
        acc = {}
        ctr = [0]

        def tick():
            for dd in list(deferred):
                dd[0] -= 1
                if dd[0] <= 0:
                    deferred.remove(dd)
                    dd[1]()

        def front(blk):
            kind, h, kb, nkb = blk
            i = ctr[0]
            ctr[0] += 1
            bS = PS.alloc()
            pt = PT[i % 4]
            if kind == "mla":
                jl = kb - 4 * t
                q0 = jl * 128 if jl > 0 else 0
                MM(ps[bS][:, q0:TB], kfT[0:96, h, kb * 128:(kb + 1) * 128], qf[0:96, h, q0:TB], True, True,
                   [("kfT", kb // 4, h), ("kpe", kb // 4), ("qf", h), "R1"], [PK(bS)])
                ACT(pt[:, q0:TB], ps[bS][:, q0:TB], AF.Exp, [PK(bS)], [("PT", i % 4)], scale=sc_mla)
                if jl >= 0:
                    TT(pt[:, q0:q0 + 128], pt[:, q0:q0 + 128], cmask[:], ALU.mult, [("PT", i % 4), "cmask"],
                       [("PT", i % 4)])
            else:
                q0 = 0
                cc, r0 = h // 2, (h % 2) * 64
                MM(ps[bS][:, :], kmT[r0:r0 + 64, cc, kb * 128:(kb + 1) * 128], qm[r0:r0 + 64, cc, :], True, True,
                   ["kmT", "qm"], [PK(bS)])
                ACT(pt[:, :], ps[bS][:, :], AF.Exp, [PK(bS)], [("PT", i % 4)], scale=sc_mem)
            PS.release(bS)
            pending.append((blk, i, q0))

        def back():
            (kind, h, kb, nkb), i, q0 = pending.pop(0)
            pt = PT[i % 4]
            if kb == 0:
                acc[(kind, h)] = PS.alloc()
            bO = acc[(kind, h)]
            if kind == "mla":
                MM(ps[bO][0:65, q0:TB], Vaug[:, kb, h, :], pt[:, q0:TB], kb == 0, kb == nkb - 1,
                   VR + [("PT", i % 4)], [PK(bO)])
            else:
                MM(ps[bO][0:65, :], vmaug[:, kb, h, :], pt[:, :], kb == 0, kb == nkb - 1,
                   ["vmaug", ("PT", i % 4)], [PK(bO)])
            if kb == nkb - 1:
                del acc[(kind, h)]
                dk = ("drow", h % 2)
                dr = drow if h % 2 == 0 else rec2
                ACT(dr[64:65, :], ps[bO][64:65, :], AF.Copy, [PK(bO)], [dk])

                def fin(kind=kind, h=h, bO=bO, dr=dr, dk=dk):
                    bD = PS.alloc()
                    MM(ps[bD][0:64, :], onesf[64:65, 0:64], dr[64:65, :], True, True, [dk, "onesf"], [PK(bD)])
                    dst, dkey = (omla, "R1b") if kind == "mla" else (omem, "omem")
                    P.add("dve", lambda e, o=rec[0:64, :], i_=ps[bD][0:64, :]: e.reciprocal(o, i_), [PK(bD)], ["rec"])
                    PS.release(bD)
                    TT(dst[0:64, h, :], ps[bO][0:64, :], rec[0:64, :], ALU.mult, [PK(bO), "rec"], [dkey])
                    PS.release(bO)

                deferred.append([2, fin])

        def push(blk):
            front(blk)
            tick()
            if len(pending) > LA:
                back()

        def attn_head(h, gen):
            nkb = 4 * (t + 1)
            done = 0
            for kb in range(nkb):
                push(("mla", h, kb, nkb))
                if gen is not None:
                    if cfg.get("two_stage", 0):
                        want = 2 if kb < int(nkb * cfg.get("two_stage_frac", 0.5)) else NSTAGE
                    else:
                        span = max(1, int(nkb * cfg.get("prep_span", 0.01)))
                        want = min(NSTAGE, ((kb + 1) * NSTAGE + span - 1) // span)
                    while done < want:
                        if next(gen, "END") == "END":
                            done = NSTAGE
                            break
                        done += 1
            if gen is not None:
                for _ in gen:
                    pass

        if (t == 0 and cfg.get("prep_first", 0)) or cfg.get("prep_first_all", 0):
            nxt = 0
            active = []
            while nxt < NH or active:
                if nxt < NH and (not active or (len(active) < 2 and active[0][1] >= 3)):
                    active.append([prep_stages(nxt), 0])
                    nxt += 1
                for a_ in list(active):
                    if next(a_[0], "END") == "END":
                        active.remove(a_)
                    else:
                        a_[1] += 1
            for h in range(NH):
                attn_head(h, None)
        else:
            g0 = prep_stages(0)
            for _ in g0:
                pass
            WARM(cfg.get("warm_attn", 0))
            for h in range(NH):
                attn_head(h, prep_stages(h + 1) if h + 1 < NH else None)
        for h in range(NMH):
            for kb in range(2):
                push(("mem", h, kb, 2))
        while pending:
            back()
            tick()
        while deferred:
            tick()
        if t == 0:
            DUMP(3, qm[:], ["qm"])
            DUMP(4, ocn[:], ["ocn"])
            DUMP(5, qf[0:96, :, :], [("qf", h) for h in range(NH)])
            DUMP(6, kfT[0:96, :, 0:TB], [("kfT", 0, h) for h in range(NH)] + [("kpe", 0)])
            DUMP(7, Vaug[:, 0:4, :, :].rearrange("p a h d -> p a (h d)"), [("Vaug", 0)])
            DUMP(8, kmT[:], ["kmT"])
            DUMP(9, vmaug[:].rearrange("p a h d -> p a (h d)"), ["vmaug"])

    def emit_attention(s, l, t):
        return

    def emit_outproj(s, l, t, last_layer_no_ffn=False):
        c0 = t * TB
        if t == 0:
            DUMP(10, omla[0:64, :, :], ["R1b"])
            DUMP(11, omem[0:64, :, :], ["omem"])
        for (buf, key, nh, ones_m, gbase) in ((omla, "R1b", NH, ones512, G_OMLA), (omem, "omem", NMH, ones256, G_OMEM)):
            ACT(SQ[0:64, 0:nh, :], buf[0:64, :, :], AF.Square, [key], ["R1"])
            b = PS.alloc()
            for h in range(nh):
                MM(ps[b][0:64, :], ones_m[0:64, 0:64], SQ[0:64, h, :], h == 0, h == nh - 1, ["R1"], [PK(b)])
            RSTD(rstd[0][0:64, :], ps[b][0:64, :], [PK(b)], ["rstd0"])
            PS.release(b)
            for h in range(nh):
                STT(buf[0:64, h, :], buf[0:64, h, :], gcol(l, gbase + h, 0, 64), rstd[0][0:64, :], ALU.mult, ALU.mult,
                    [key, "rstd0", "gains"], [key])
        if t == 0:
            DUMP(12, omla[0:64, :, :], ["R1b"])
            DUMP(13, omem[0:64, :, :], ["omem"])
        WARM(cfg.get("warm_out", 32))
        for m in range(KC):
            b = PS.alloc()
            nmm = NH + NMH + 2
            i = 0
            for h in range(NH):
                MM(ps[b][:], wo_mla[0:64, h, m * 128:(m + 1) * 128], omla[0:64, h, :], i == 0, i == nmm - 1,
                   ["wo", "R1b"], [PK(b)])
                i += 1
            for h in range(NMH):
                MM(ps[b][:], wo_mem[0:64, h, m * 128:(m + 1) * 128], omem[0:64, h, :], i == 0, i == nmm - 1,
                   ["wo", "omem"], [PK(b)])
                i += 1
            for cc in range(2):
                MM(ps[b][:], wo_conv[:, cc, m * 128:(m + 1) * 128], ocn[:, cc, :], i == 0, i == nmm - 1,
                   ["wo", "ocn"], [PK(b)])
                i += 1
            TT(xt[:, m, :], xt[:, m, :], ps[b][:], ALU.add, [("xt", m), PK(b)], [("xt", m)])
            PS.release(b)
            DMA("sp", xs_d[s, m, :, c0:c0 + TB], xt[:, m, :], [("xt", m)], [("xs", s)])


    def emit_ffn(s, l, final, prefetch_l=None):
        for t in range(NTB):
            for c in range(KC):
                DMA("sp", XF[:, c, t * TB:(t + 1) * TB], xs_d[s, c, :, t * TB:(t + 1) * TB], [("xs", s)], [("XFl", c, t)])
        XFK = [("XF", c) for c in range(KC)]
        moe = (l % 2 == 1)
        skip = (n_units_cap == 0)
        if moe and not skip:
            DMA("sp", wr_sb[:], wr_d[0].rearrange("(k p) e -> p k e", p=128), (), ["wr_sb"])
            for k in range(KC):
                TS(wrg[:, k, :], wr_sb[:, k, :], gcol(l, G_FFN + k), None, ALU.mult, None, ["wr_sb", "gains"], ["wrg"])
        for t in range(0 if not skip else NTB, NTB):
            c0 = t * TB
            ACT(FSQ[:], XF[:, :, c0:c0 + TB], AF.Square, XFK + [("XFl", c, t) for c in range(KC)], ["FSQ"])
            b = PS.alloc()
            for k in range(KC):
                MM(ps[b][:], ones1024[:], FSQ[:, k, :], k == 0, k == KC - 1, ["FSQ", "ones1024"], [PK(b)])
            RSTD(frstd[:], ps[b][:], [PK(b)], ["frstd"])
            PS.release(b)
            for k in range(KC):
                STT(HT[:, k, c0:c0 + TB], XF[:, k, c0:c0 + TB], gcol(l, G_FFN + k), frstd[:], ALU.mult, ALU.mult,
                    [("XF", k), ("XFl", k, t), "frstd", "gains"], [("HT", t)])
            if moe:
                ACT(combT[32:33, c0:c0 + TB], frstd[32:33, :], AF.Copy, ["frstd"], ["rsrow"])

        rt_state = {"bT": None, "pend": []}
        RT = (rt, rt2, rt3)

        def router_transpose(jb):
            t_, j = jb // 4, jb % 4
            r_ = RT[jb % 3]
            rk = ("rt", jb % 3)
            if j == 0:
                rt_state["bT"] = PS.alloc()
            bT = rt_state["bT"]
            TR(ps[bT][0:8, j * 128:(j + 1) * 128], r_[:, 48:56], [rk, "ident"], [PK(bT)])
            if j == 3:
                ACT(combT[0:8, t_ * TB:(t_ + 1) * TB], ps[bT][0:8, :], AF.Copy, [PK(bT)], ["combT"])
                PS.release(bT)

        def router_block(jb):
            if len(rt_state["pend"]) >= 2:
                router_transpose(rt_state["pend"].pop(0))
            r_ = RT[jb % 3]
            rk = ("rt", jb % 3)
            tok = slice(jb * 128, (jb + 1) * 128)
            b = PS.alloc()
            for k in range(KC):
                MM(ps[b][:, 0:8], XF[:, k, tok], wrg[:, k, :], k == 0, k == KC - 1, [("XF", k), "wrg"], [PK(b)])
            MM(ps[b][:, 8:9], combT[32:33, tok], onesf[32:33, 0:1], True, True, ["rsrow", "onesf"], [PK(b)])
            ACT(r_[:, 8:9], ps[b][:, 8:9], AF.Copy, [PK(b)], [rk])
            TS(r_[:, 0:8], ps[b][:, 0:8], r_[:, 8:9], None, ALU.mult, None, [PK(b), rk], [rk])
            PS.release(b)
            P.add("dve", lambda e: e.max(r_[:, 16:24], r_[:, 0:8]), [rk], [rk])
            TS(r_[:, 24:32], r_[:, 0:8], r_[:, 16:17], None, ALU.subtract, None, [rk], [rk])
            ACT(r_[:, 24:32], r_[:, 24:32], AF.Exp, [rk], [rk])
            TS(r_[:, 32:40], r_[:, 0:8], r_[:, 17:18], None, ALU.is_ge, None, [rk], [rk])
            TT(r_[:, 24:32], r_[:, 24:32], r_[:, 32:40], ALU.mult, [rk], [rk])
            P.add("dve", lambda e: e.reduce_sum(r_[:, 40:41], r_[:, 24:32], AX.X), [rk], [rk])
            P.add("dve", lambda e: e.reciprocal(r_[:, 41:42], r_[:, 40:41]), [rk], [rk])
            TS(r_[:, 48:56], r_[:, 24:32], r_[:, 41:42], None, ALU.mult, None, [rk], [rk])
            rt_state["pend"].append(jb)

        def router_flush():
            while rt_state["pend"]:
                router_transpose(rt_state["pend"].pop(0))

        router_todo = list(range(S // 128)) if (moe and not skip) else []
        if not cfg.get("router_overlap", 1):
            while router_todo:
                router_block(router_todo.pop(0))
            router_flush()
        if not moe:
            li = l // 2
            units = [(wdgu_d[li], 0, 2816, wdd_d[li], 0, None), (wdgu_d[li], EFF, 2816 + EFF, wdd_d[li], EFF, None)]
        else:
            li = l // 2
            units = [(wegu_d[li, e], 0, EFF, wed_d[li, e], 0, e) for e in range(8)]
        units = units[:n_units_cap]
        gu_i = [0]
        dw_i = [0]
        HTK = [("HT", t) for t in range(NTB)]
        for ui, (wgu, gc0, uc0, wd, dr0, ex) in enumerate(units):
            def emit_cw(ex=ex):
                for t in range(NTB):
                    b = PS.alloc()
                    MM(ps[b][:], selT[0:8, ex, :], combT[0:8, t * TB:(t + 1) * TB], True, True, ["selT", "combT"], [PK(b)])
                    ACT(CW[:, t * TB:(t + 1) * TB], ps[b][:], AF.Copy, [PK(b)], ["CW"])
                    PS.release(b)

            cw_late = ex is not None and bool(router_todo)
            if ex is not None and not cw_late:
                emit_cw()
            for j in range(NJ):
                sl = gu_i[0] % NGU
                gu_i[0] += 1
                DMA("pool", gu[sl][:, :, 0:128],
                    wgu[:, gc0 + j * 128: gc0 + (j + 1) * 128].rearrange("(k p) n -> p k n", p=128), (), [("gu", sl)])
                DMA("pool", gu[sl][:, :, 128:256],
                    wgu[:, uc0 + j * 128: uc0 + (j + 1) * 128].rearrange("(k p) n -> p k n", p=128), (), [("gu", sl)])
                for t in range(NTB):
                    bg = PS.alloc()
                    bu = PS.alloc()
                    for k in range(KC):
                        MM(ps[bg][:], gu[sl][:, k, 0:128], HT[:, k, t * TB:(t + 1) * TB], k == 0, k == KC - 1,
                           [("gu", sl), ("HT", t)], [PK(bg)])
                    for k in range(KC):
                        MM(ps[bu][:], gu[sl][:, k, 128:256], HT[:, k, t * TB:(t + 1) * TB], k == 0, k == KC - 1,
                           [("gu", sl), ("HT", t)], [PK(bu)])
                    si = (j * NTB + t) % 2
                    ACT(ssb[si][:], ps[bg][:], AF.Silu, [PK(bg)], [("ssb", si)])
                    PS.release(bg)
                    TT(ABUF[:, j, t * TB:(t + 1) * TB], ssb[si][:], ps[bu][:], ALU.mult, [("ssb", si), PK(bu)],
                       [("A", j, t)])
                    PS.release(bu)
                for _ in range(2):
                    if router_todo:
                        router_block(router_todo.pop(0))
            while router_todo:
                router_block(router_todo.pop(0))
            router_flush()
            if cw_late:
                emit_cw()
            for m in range(KC):
                if m == NDW and prefetch_l is not None and ui == len(units) - 1:
                    emit_mixer_weights_main(prefetch_l, prefetch=True)
                sl = dw_i[0] % NDW
                dw_i[0] += 1
                DMA("pool", dw[sl][:], wd[dr0:dr0 + EFF, m * 128:(m + 1) * 128].rearrange("(j p) n -> p j n", p=128),
                    (), [("dw", sl)])
                for t in range(NTB):
                    b = PS.alloc()
                    for j in range(NJ):
                        MM(ps[b][:], dw[sl][:, j, :], ABUF[:, j, t * TB:(t + 1) * TB], j == 0, j == NJ - 1,
                           [("dw", sl), ("A", j, t)], [PK(b)])
                    xsl = XF[:, m, t * TB:(t + 1) * TB]
                    if ex is not None:
                        ti = (m * NTB + t) % 2
                        TT(tmpb[ti][:], ps[b][:], CW[:, t * TB:(t + 1) * TB], ALU.mult, [PK(b), "CW"], [("tmpb", ti)])
                        TT(xsl, xsl, tmpb[ti][:], ALU.add, [("XF", m), ("tmpb", ti)], [("XF", m)])
                    else:
                        TT(xsl, xsl, ps[b][:], ALU.add, [("XF", m), PK(b)], [("XF", m)])
                    PS.release(b)
        if final:
            outs = []
            for tk in range(S // 128):
                for half in range(2):
                    b = PS.alloc()
                    for ci in range(4):
                        c = half * 4 + ci
                        TR(ps[b][:, ci * 128:(ci + 1) * 128], XF[:, c, tk * 128:(tk + 1) * 128],
                           [("XF", c), ("XFl", c, tk // 4), "ident"], [PK(b)])
                    if half == 0:
                        ACT(ostage[:, 0:512], ps[b][:], AF.Copy, [PK(b)] + [("A", j, t) for j in range(4) for t in range(NTB)],
                            ["ostage"])
                    else:
                        P.add("dve", lambda e, o=ostage[:, 512:1024], i_=ps[b][:]: e.tensor_copy(o, i_),
                              [PK(b)] + [("A", j, t) for j in range(4) for t in range(NTB)], ["ostage"])
                    PS.release(b)
                outs.append(DMA("sp", y_d[s, tk * 128:(tk + 1) * 128, :], ostage[:], ["ostage"], [("y", s)]))
            return outs
        else:
            for c in range(KC):
                DMA("sp", xs_d[s, c, :, :], XF[:, c, :], [("XF", c)], [("xs", s)])
            return []

    have_main = [False]
    early = set()
    if do_mixer and cfg.get("early_w", 1):
        n0 = len(P.ops)
        emit_mixer_weights_main(0)
        early = set(P.ops[n0:])
        have_main[0] = True
    emit_init()
    P.fence(exclude=early)
    out_dmas = []
    for s in range(n_seq):
        for l in range(n_layers):
            if do_mixer:
                emit_mixer_weights(l, have_main[0])
                have_main[0] = False
                emit_mem_path(s, l)
                P.fence()
                for t in range(ntb_cap):
                    emit_load_xt(s, l, t)
                    emit_m1(s, l, t)
                    emit_attention(s, l, t)
                    emit_outproj(s, l, t)
            else:
                for t in range(NTB):
                    emit_load_xt(s, l, t)
                    for m in range(KC):
                        DMA("sp", xs_d[s, m, :, t * TB:(t + 1) * TB], xt[:, m, :], [("xt", m)], [("xs", s)])
            P.fence()
            final = (l == n_layers - 1)
            is_last = (s == n_seq - 1 and l == n_layers - 1)
            pf = None if (is_last or not do_mixer or not cfg.get("prefetch", 1) or n_units_cap == 0) else (l + 1) % n_layers
            out_dmas += emit_ffn(s, l, final, pf)
            have_main[0] = pf is not None
            P.fence()
    fin = P.add("sp", None, (), ())
    for o in out_dmas:
        fin.deps.add(o)
    P.finalize()

    sem_names = list(P.semcount.keys())
    import contextlib
    with contextlib.ExitStack() as st:
        sems = {nm: st.enter_context(nc.semaphore("s_" + nm)) for nm in sem_names}
        block = st.enter_context(nc.Block())

        @block.tensor
        def _(e):
            P.replay("pe", e, sems)

        @block.scalar
        def _(e):
            P.replay("act", e, sems)

        @block.vector
        def _(e):
            P.replay("dve", e, sems)

        @block.gpsimd
        def _(e):
            P.replay("pool", e, sems)

        @block.sync
        def _(e):
            P.replay("sp", e, sems)

    return nc, P


def prep_weights(inp):
    f = lambda a: np.ascontiguousarray(np.asarray(a, dtype=np.float32))
    w_in = f(inp["w_in"])
    kpe = w_in[:, :, 384:416]
    w_in_ext = np.concatenate([w_in, kpe[:, :, 16:32], kpe[:, :, 0:16]], axis=2)
    wq = f(inp["w_q_up"]).reshape(2, 256, NH, 96)
    ext = np.concatenate([wq[..., 0:64], wq[..., 80:96], wq[..., 64:80]], axis=-1)
    w_q_ext = np.concatenate([wq.reshape(2, 256, 768), ext.reshape(2, 256, 768)], axis=2)
    G = np.zeros((2, 128, NG), np.float32)
    for l in range(2):
        def colk(v, n):
            return np.asarray(v, np.float32).reshape(n, 128).T
        G[l, :, G_MIX:G_MIX + 8] = colk(inp["g_mix"][l], 8)
        G[l, :, G_FFN:G_FFN + 8] = colk(inp["g_ffn"][l], 8)
        G[l, :, G_QLAT:G_QLAT + 2] = colk(inp["g_q_lat"][l], 2)
        G[l, :, G_KVLAT] = inp["g_kv_lat"][l]
        gq = np.asarray(inp["g_q_mla"][l], np.float32)
        gk = np.asarray(inp["g_k_mla"][l], np.float32)
        G[l, 0:96, G_Q96] = gq
        G[l, 64:96, G_Q96SW] = np.concatenate([gq[80:96], gq[64:80]])
        G[l, 0:96, G_K96] = gk
        G[l, 64:96, G_K96SW] = np.concatenate([gk[80:96], gk[64:80]])
        G[l, :, G_QMEM] = np.tile(np.asarray(inp["g_q_mem"][l], np.float32), 2)
        G[l, :, G_KMEM] = np.tile(np.asarray(inp["g_k_mem"][l], np.float32), 2)
        go = np.asarray(inp["g_out"][l], np.float32)
        G[l, 0:64, G_OMLA:G_OMLA + 8] = go[0:512].reshape(8, 64).T
        G[l, 0:64, G_OMEM:G_OMEM + 4] = go[512:768].reshape(4, 64).T
        G[l, :, G_OCONV:G_OCONV + 2] = go[768:1024].reshape(2, 128).T
        cw = np.asarray(inp["conv_w"][l], np.float32)
        for tap in range(3):
            G[l, :, G_CONVW + tap * 2:G_CONVW + tap * 2 + 2] = cw[tap].reshape(2, 128).T
        G[l, :, G_MEM:G_MEM + 8] = colk(inp["g_mem"][l], 8)
    return {
        "w_in_ext": np.ascontiguousarray(w_in_ext),
        "w_q_ext": np.ascontiguousarray(w_q_ext),
        "w_kv_up": f(inp["w_kv_up"]),
        "w_mem_kv": f(inp["w_mem_kv"]),
        "w_out": f(inp["w_out"]),
        "w_dense_gu": f(inp["w_dense_gu"]),
        "w_dense_down": f(inp["w_dense_down"]),
        "w_router": f(inp["w_router"]),
        "w_expert_gu": f(inp["w_expert_gu"]),
        "w_expert_down": f(inp["w_expert_down"]),
        "gains": G,
    }


_CACHE = {}


def kernel(**inputs):
    x = np.asarray(inputs["x"], np.float32)
    mem = np.asarray(inputs["mem"], np.float32)
    shared = prep_weights(inputs)
    if "nc" not in _CACHE:
        _CACHE["nc"] = build_program()[0]
    nc = _CACHE["nc"]
    in_maps = []
    for c in range(NCORES):
        m = dict(shared)
        m["x"] = np.ascontiguousarray(x[c * SEQ_PER_CORE:(c + 1) * SEQ_PER_CORE])
        m["mem"] = np.ascontiguousarray(mem[c * SEQ_PER_CORE:(c + 1) * SEQ_PER_CORE])
        in_maps.append(m)
    res = run_bass_kernel_spmd(nc, in_maps, core_ids=list(range(NCORES)))
    out = np.concatenate([np.asarray(r["y"], np.float32) for r in res.results], axis=0)
    return out
```

```python
import math
import numpy as np
import concourse.bass as bass
import concourse.mybir as mybir
from concourse.bass_utils import run_bass_kernel_spmd

F32 = mybir.dt.float32
BF16 = mybir.dt.bfloat16
ALU = mybir.AluOpType
AF = mybir.ActivationFunctionType
AX = mybir.AxisListType

NCORES = 8
SEQ_PER_CORE = 2
S = 2048
D = 1024
KC = 8
TB = 512
NTB = S // TB
MEMT = 256
EPS = 1e-6
NH = 8
NMH = 4
EFF = 1408
NJ = EFF // 128
WIN_COLS = 1472
WQ_COLS = 1536

G_MIX, G_FFN, G_QLAT, G_KVLAT, G_Q96, G_Q96SW, G_K96, G_K96SW, G_QMEM, G_KMEM = 0, 8, 16, 18, 19, 20, 21, 22, 23, 24
G_OMLA, G_OMEM, G_OCONV, G_CONVW, G_MEM = 25, 33, 37, 39, 45
NG = 53


class Op:
    __slots__ = ("q", "sem", "fn", "deps", "qidx", "semidx", "waits", "signal", "sigval", "clock", "inc")


class Prog:
    QUEUES = ("pe", "act", "dve", "pool", "sp")
    NDMA = 12

    def __init__(self):
        self.ops = []
        self.qops = {q: [] for q in self.QUEUES}
        self.semcount = {}
        self.lastw = {}
        self.readers = {}
        self.dma_rr = {}
        self.last_on_sem = {}

    def add(self, q, fn, reads=(), writes=(), dma=False):
        op = Op()
        op.q = q
        op.fn = fn
        op.signal = False
        op.waits = []
        deps = set()
        pk = [k for k in reads if isinstance(k, str) and k.startswith("ps")]
        if pk:
            reads = [k for k in reads if k not in pk]
            writes = list(writes) + pk
        for k in reads:
            w = self.lastw.get(k)
            if w is not None:
                deps.add(w)
        for k in writes:
            w = self.lastw.get(k)
            if w is not None:
                deps.add(w)
            for r in self.readers.get(k, ()):
                deps.add(r)
        for k in reads:
            self.readers.setdefault(k, []).append(op)
        for k in writes:
            self.lastw[k] = op
            self.readers[k] = []
        if dma:
            rr = self.dma_rr.get(q, 0)
            op.sem = "dma_%s_%d" % (q, rr % self.NDMA)
            self.dma_rr[q] = rr + 1
            op.inc = 16
            op.signal = True
            prev = self.last_on_sem.get(op.sem)
            if prev is not None:
                deps.add(prev)
        else:
            op.sem = q
            op.inc = 1
        self.last_on_sem[op.sem] = op
        deps.discard(op)
        op.deps = deps
        op.qidx = len(self.qops[q])
        op.semidx = self.semcount.get(op.sem, 0)
        self.semcount[op.sem] = op.semidx + 1
        self.qops[q].append(op)
        self.ops.append(op)
        return op

    def fence(self, exclude=()):
        for _ in range(2):
            lasts = [o for o in self.last_on_sem.values() if o not in exclude]
            for q in self.QUEUES:
                op = self.add(q, None, (), ())
                for l in lasts:
                    if l is not op:
                        op.deps.add(l)

    def finalize(self):
        clocks = {q: {} for q in self.QUEUES}
        for op in self.ops:
            clk = clocks[op.q]
            need = {}
            for d in op.deps:
                if d.sem == op.q:
                    if op.q == "pe":
                        continue
                    if op.qidx - d.qidx >= 3:
                        continue
                if clk.get(d.sem, -1) >= d.semidx:
                    continue
                cur = need.get(d.sem)
                if cur is None or cur.semidx < d.semidx:
                    need[d.sem] = d
            if need:
                clk = dict(clk)
                for d in need.values():
                    d.signal = True
                    for s, v in d.clock.items():
                        if clk.get(s, -1) < v:
                            clk[s] = v
                    if clk.get(d.sem, -1) < d.semidx:
                        clk[d.sem] = d.semidx
                clocks[op.q] = clk
                op.waits = list(need.values())
            op.clock = clk
        cnt = {}
        for op in self.ops:
            if op.signal:
                cnt[op.sem] = cnt.get(op.sem, 0) + op.inc
                op.sigval = cnt[op.sem]

    def replay(self, q, e, sems):
        for op in self.qops[q]:
            for d in op.waits:
                e.wait_ge(sems[d.sem], d.sigval)
            if op.fn is None:
                if op.signal:
                    e.nop(nofuse=True).then_inc(sems[op.sem], op.inc)
                continue
            ins = op.fn(e)
            if op.signal:
                ins.then_inc(sems[op.sem], op.inc)


class PsumPool:
    def __init__(self, n):
        self.free = list(range(n))

    def alloc(self):
        assert self.free, "psum exhausted"
        return self.free.pop(0)

    def release(self, b):
        self.free.append(b)


class Arena:
    BASE = 16512
    END = 229376

    def __init__(self, nc):
        self.nc = nc
        self.cur = self.BASE
        self.n = 0
        self.off = {}

    def alloc(self, name, shape, dt, at=None):
        nbytes = int(np.prod(shape[1:])) * (2 if dt == BF16 else 4)
        nbytes = (nbytes + 63) // 64 * 64
        if at is None:
            off = self.cur
            self.cur += nbytes
        else:
            off = at
        assert off + nbytes <= self.END, ("sbuf overflow", name, off, nbytes)
        self.n += 1
        self.off[name] = off
        return self.nc.alloc_sbuf_tensor_at("%s_%d" % (name, self.n), list(shape), dt, offset=off)


def build_program(cfg=None):
    cfg = cfg or {}
    n_seq = cfg.get("n_seq", SEQ_PER_CORE)
    n_layers = cfg.get("n_layers", 2)
    do_mixer = cfg.get("mixer", True)
    do_ffn = cfg.get("ffn", True)
    n_units_cap = cfg.get("n_units", 99)

    nc = bass.Bass("TRN2", target_bir_lowering=False)
    for fn, why in ((getattr(nc, "allow_low_precision", None), "bf16 matmul operands, fp32 accumulation"),
                    (getattr(nc, "allow_non_contiguous_dma", None), "weight re-layout")):
        if fn is not None:
            try:
                fn(why)
            except Exception:
                pass
    P = Prog()
    PS = PsumPool(8)

    def din(name, shape):
        return nc.dram_tensor(name, list(shape), F32, kind="ExternalInput").ap()

    x_d = din("x", [SEQ_PER_CORE, S, D])
    mem_d = din("mem", [SEQ_PER_CORE, MEMT, D])
    win_d = din("w_in_ext", [2, D, WIN_COLS])
    wq_d = din("w_q_ext", [2, 256, WQ_COLS])
    wkv_d = din("w_kv_up", [2, 128, 1024])
    wmem_d = din("w_mem_kv", [2, D, 512])
    wout_d = din("w_out", [2, D, D])
    wdgu_d = din("w_dense_gu", [1, D, 5632])
    wdd_d = din("w_dense_down", [1, 2816, D])
    wr_d = din("w_router", [1, D, 8])
    wegu_d = din("w_expert_gu", [1, 8, D, 2816])
    wed_d = din("w_expert_down", [1, 8, EFF, D])
    gains_d = din("gains", [2, 128, NG])
    y_d = nc.dram_tensor("y", [SEQ_PER_CORE, S, D], F32, kind="ExternalOutput").ap()
    xs_d = nc.dram_tensor("xs_scratch", [SEQ_PER_CORE, KC, 128, S], F32).ap()

    ps = [nc.alloc_psum_tensor("psb%d" % i, [128, 512], F32) for i in range(8)]
    NDUMP = 24
    dump_on = bool(cfg.get("dump"))
    dbg_d = nc.dram_tensor("dbg", [NDUMP, 128, 4096], F32, kind="ExternalOutput").ap() if dump_on else None
    ntb_cap = cfg.get("ntb", NTB)

    def PK(b):
        return "ps%d" % b

    A = Arena(nc)
    ident = A.alloc("ident", [128, 128], F32)
    onesf = A.alloc("onesf", [128, 64], F32)
    epsc = A.alloc("epsc", [128, 8], F32)
    ones1024 = A.alloc("ones1024", [128, 128], BF16)
    ones512 = A.alloc("ones512", [128, 128], BF16)
    ones256 = A.alloc("ones256", [128, 128], BF16)
    ones128 = A.alloc("ones128", [128, 128], BF16)
    blk96 = A.alloc("blk96", [128, 128], BF16)
    blk64 = A.alloc("blk64", [128, 128], BF16)
    cmask = A.alloc("cmask", [128, 128], BF16)
    selT = A.alloc("selT", [8, 8, 128], F32)
    pidx = A.alloc("pidx", [128, 8], F32)
    colidx = A.alloc("colidx", [128, 128], F32)
    gains = A.alloc("gains", [128, 2, NG], F32)
    COS = A.alloc("COS", [128, S], BF16)
    SINS = A.alloc("SINS", [128, S], BF16)
    PERS_END = A.cur

    w_in = A.alloc("w_in", [128, KC, WIN_COLS], BF16)
    w_q = A.alloc("w_q", [128, 2, WQ_COLS], BF16)
    w_kv = A.alloc("w_kv", [128, 1024], BF16)
    wo_mla = A.alloc("wo_mla", [64, 8, D], BF16)
    wo_mem = A.alloc("wo_mem", [64, 4, D], BF16)
    wo_conv = A.alloc("wo_conv", [128, 2, D], BF16)
    kfT = A.alloc("kfT", [96, NH, S], BF16)
    Vaug = A.alloc("Vaug", [128, S // 128, NH, 65], BF16)
    kmT = A.alloc("kmT", [128, 2, MEMT], BF16)
    vmaug = A.alloc("vmaug", [128, 2, NMH, 65], BF16)
    xt = A.alloc("xt", [128, KC, TB], F32)
    stage = A.alloc("stage", [128, 4, D], F32)
    R1 = A.off["stage"]
    SQ = A.alloc("SQ", [128, KC, TB], BF16, at=R1)
    hT = A.alloc("hT", [128, KC, TB], BF16, at=R1 + 8192)
    qf = A.alloc("qf", [96, NH, TB], BF16, at=R1)
    omla = A.alloc("omla", [64, NH, TB], BF16, at=R1 + 8192)
    qlat = A.alloc("qlat", [128, 2, TB], F32)
    R2 = A.off["qlat"]
    kvlat = A.alloc("kvlat", [128, TB], F32)
    t1 = A.alloc("t1", [128, TB], F32, at=R2)
    t2 = A.alloc("t2", [128, TB], F32, at=R2 + 2048)
    r96 = A.alloc("r96", [128, TB], F32, at=R2 + 4096)
    u_sb = A.alloc("u_sb", [128, TB], F32)
    R3 = A.off["u_sb"]
    ybuf = A.alloc("ybuf", [128, TB], F32)
    oconv = A.alloc("oconv", [128, 2, TB], F32)
    R3_END = A.cur
    PT = [A.alloc("PT%d" % i, [128, TB], BF16, at=R3 + i * 1024) for i in range(4)]
    drow = A.alloc("drow", [128, TB], F32, at=R3 + 4096)
    rec = A.alloc("rec", [128, TB], F32, at=R3 + 6144)
    rec2 = A.alloc("rec2", [128, TB], F32)
    rcb = None
    assert R3 + 8192 <= R3_END
    vbuf = A.alloc("vbuf", [128, 2, TB + 16], F32)
    omem = A.alloc("omem", [64, NMH, TB], BF16)
    sq2 = A.alloc("sq2", [128, 2, TB], BF16)
    qlatn = A.alloc("qlatn", [128, 2, TB], BF16)
    kvlatn = A.alloc("kvlatn", [128, TB], BF16)
    rstd = [A.alloc("rstd%d" % i, [128, TB], F32) for i in range(2)]
    sq96 = A.alloc("sq96", [128, TB], BF16)
    sq96b = A.alloc("sq96b", [128, TB], BF16)
    t1b = A.alloc("t1b", [128, TB], F32)
    t2b = A.alloc("t2b", [128, TB], F32)
    r96b = A.alloc("r96b", [128, TB], F32)
    qm = A.alloc("qm", [128, 2, TB], BF16)
    ocn = A.alloc("ocn", [128, 2, TB], BF16)
    MIX_END = A.cur
    XT0 = A.off["xt"]
    memtok = A.alloc("memtok", [128, 2, D], F32, at=XT0)
    wmem = A.alloc("wmem", [128, KC, 512], BF16, at=XT0 + 8192)
    memT = A.alloc("memT", [128, KC, MEMT], BF16, at=XT0 + 16384)
    msq = A.alloc("msq", [128, D], BF16, at=XT0 + 20480)
    mss = A.alloc("mss", [128, 8], F32, at=XT0 + 22528)
    KF0 = A.off["kfT"]
    itmp = [A.alloc("itmp%d" % i, [128, S], F32, at=KF0 + i * 8192) for i in range(3)]
    iint = A.alloc("iint", [128, S], mybir.dt.int32, at=KF0 + 3 * 8192)
    pint = A.alloc("pint", [128, 8], mybir.dt.int32, at=KF0 + 4 * 8192)

    A.cur = PERS_END
    HT = A.alloc("HT", [128, KC, S], BF16)
    NGU, NDW = 4, 3
    gu = [A.alloc("gu%d" % i, [128, KC, 256], BF16) for i in range(NGU)]
    assert A.cur >= A.off["wo_mla"] + 16384, "prefetched mixer weights must lie inside HT+gu"
    XF = A.alloc("XF", [128, KC, S], F32)
    ABUF = A.alloc("ABUF", [128, NJ, S], BF16)
    AB0 = A.off["ABUF"]
    FSQ = A.alloc("FSQ", [128, KC, TB], BF16, at=AB0)
    ostages = [A.alloc("ostage%d" % i, [128, D], F32, at=AB0 + 8192 + i * 4096) for i in range(4)]
    CW = A.alloc("CW", [128, S], F32)
    tmpb = [A.alloc("tmpb%d" % i, [128, TB], F32) for i in range(2)]
    ssb = [A.alloc("ssb%d" % i, [128, TB], F32) for i in range(2)]
    dw = [A.alloc("dw%d" % i, [128, NJ, 128], BF16) for i in range(NDW)]
    frstd = A.alloc("frstd", [128, TB], F32)
    combT = A.alloc("combT", [40, S], F32)
    wr_sb = A.alloc("wr_sb", [128, KC, 8], F32)
    wrg = A.alloc("wrg", [128, KC, 8], F32)
    rt = A.alloc("rt", [128, 64], F32)
    rt2 = A.alloc("rt2", [128, 64], F32)
    rt3 = A.alloc("rt3", [128, 64], F32)
    FFN_END = A.cur
    if cfg.get("verbose"):
        print("SBUF: pers_end", PERS_END, "mix_end", MIX_END, "ffn_end", FFN_END, "limit", Arena.END)

    def MM(out, lhsT, rhs, start, stop, r, w):
        P.add("pe", lambda e: e.matmul(out, lhsT, rhs, start=start, stop=stop), r, w)

    def TR(out, in_, r, w):
        P.add("pe", lambda e: e.transpose(out, in_, ident[:]), r, w)

    def ACT(out, in_, func, r, w, scale=1.0, bias=0.0, accum=None):
        if accum is None:
            P.add("act", lambda e: e.activation(out, in_, func, bias=bias, scale=scale), r, w)
        else:
            P.add("act", lambda e: e.activation(out, in_, func, bias=bias, scale=scale, accum_out=accum), r, w)

    def TS(out, in0, s1, s2, op0, op1, r, w, q="dve"):
        if op1 is None:
            P.add(q, lambda e: e.tensor_scalar(out, in0, s1, None, op0), r, w)
        else:
            P.add(q, lambda e: e.tensor_scalar(out, in0, s1, s2, op0, op1), r, w)

    def STT(out, in0, scalar, in1, op0, op1, r, w, q="dve"):
        P.add(q, lambda e: e.scalar_tensor_tensor(out, in0, scalar, in1, op0, op1), r, w)

    def TT(out, in0, in1, op, r, w, q="dve"):
        P.add(q, lambda e: e.tensor_tensor(out, in0, in1, op), r, w)

    def MEMSET(ap, val, w, q="dve"):
        P.add(q, lambda e: e.memset(ap, val), (), w)

    def DMA(q, out, in_, r, w):
        return P.add(q, lambda e: e.dma_start(out=out, in_=in_), r, w, dma=True)

    def DUMP(i, ap, keys):
        if not dump_on:
            return
        shp = ap.shape
        if len(shp) == 2:
            dst = dbg_d[i, 0:shp[0], 0:shp[1]]
        else:
            dst = dbg_d[i, 0:shp[0], 0:shp[1] * shp[2]].rearrange("p (a b) -> p a b", a=shp[1])
        DMA("pool", dst, ap, keys, [("dbg", i)])

    def WARM(n):
        if n <= 0:
            return
        bw = PS.alloc()
        for _ in range(n):
            MM(ps[bw][:], ones1024[:], COS[:, 0:TB], True, True, ["ones1024", "COS"], [PK(bw)])
        PS.release(bw)

    def RSTD(out, in_ps, r, w):
        p0 = out.base_partition()
        if cfg.get("arsqrt"):
            ACT(out, in_ps, AF.Abs_reciprocal_sqrt, r, w, bias=epsc[p0:p0 + out.shape[0], 0:1])
            return
        ACT(out, in_ps, AF.Ln, r, w, bias=epsc[p0:p0 + out.shape[0], 0:1])
        ACT(out, out, AF.Exp, w, w, scale=-0.5)

    def gcol(l, c, p0=0, p1=128):
        return gains[p0:p1, l, c:c + 1]

    def emit_init():
        P.add("pool", lambda e: e.iota(pidx[:, 0:1], [[0, 1]], base=0, channel_multiplier=1,
                                       allow_small_or_imprecise_dtypes=True), (), ["pidx"])
        P.add("pool", lambda e: e.iota(colidx[:], [[1, 128]], base=0, channel_multiplier=0,
                                       allow_small_or_imprecise_dtypes=True), (), ["colidx"])
        P.add("pool", lambda e: e.iota(itmp[0][:], [[1, S]], base=0, channel_multiplier=0,
                                       allow_small_or_imprecise_dtypes=True), (), ["itmp0"])
        P.add("pool", lambda e: e.iota(selT[:], [[1, 8], [0, 128]], base=0, channel_multiplier=0,
                                       allow_small_or_imprecise_dtypes=True), (), ["selT"])
        DMA("sp", gains[:], gains_d.rearrange("l p g -> p l g"), (), ["gains"])
        TS(ident[:], colidx[:], pidx[:, 0:1], None, ALU.is_equal, None, ["colidx", "pidx"], ["ident"])
        TS(cmask[:], colidx[:], pidx[:, 0:1], None, ALU.is_ge, None, ["colidx", "pidx"], ["cmask"])
        TS(selT[:], selT[:], pidx[0:8, 0:1], None, ALU.is_equal, None, ["selT", "pidx"], ["selT"])
        MEMSET(onesf[:], 1.0, ["onesf"])
        MEMSET(epsc[:], EPS, ["epsc"])
        MEMSET(ones1024[:], 1.0 / 1024, ["ones1024"])
        MEMSET(ones512[:], 1.0 / 512, ["ones512"])
        MEMSET(ones256[:], 1.0 / 256, ["ones256"])
        MEMSET(ones128[:], 1.0 / 128, ["ones128"])
        MEMSET(blk96[:], 0.0, ["blk96"])
        MEMSET(blk96[0:64, 0:64], 1.0 / 64, ["blk96"])
        MEMSET(blk96[64:96, 64:96], 1.0 / 32, ["blk96"])
        MEMSET(blk64[:], 0.0, ["blk64"])
        MEMSET(blk64[0:64, 0:64], 1.0 / 64, ["blk64"])
        MEMSET(blk64[64:128, 64:128], 1.0 / 64, ["blk64"])
        P.add("pool", lambda e: e.iota(pint[:, 0:1], [[0, 1]], base=0, channel_multiplier=1), (), ["pint"])
        P.add("dve", lambda e: e.tensor_single_scalar(pint[:, 1:2], pint[:, 0:1], 15, ALU.bitwise_and), ["pint"], ["pint"])
        P.add("dve", lambda e: e.tensor_single_scalar(pint[:, 2:3], pint[:, 0:1], 16, ALU.bitwise_and), ["pint"], ["pint"])
        P.add("dve", lambda e: e.tensor_copy(pidx[:, 1:2], pint[:, 1:2]), ["pint"], ["pidx"])
        P.add("dve", lambda e: e.tensor_copy(pidx[:, 3:4], pint[:, 2:3]), ["pint"], ["pidx"])
        ACT(pidx[:, 2:3], pidx[:, 1:2], AF.Exp, ["pidx"], ["pidx"], scale=-math.log(10000.0) / 16.0)
        TS(pidx[:, 4:5], pidx[:, 3:4], -1.0 / 8.0, 1.0, ALU.mult, ALU.add, ["pidx"], ["pidx"])
        two_pi = 2.0 * math.pi

        def reduce_sin(dst, shift, post_scalar_ap, post_imm):
            TS(itmp[1][:], itmp[0][:], pidx[:, 2:3], shift, ALU.mult, ALU.add, ["itmp0", "pidx"], ["itmp1"])
            TS(itmp[2][:], itmp[1][:], 1.0 / two_pi, None, ALU.mult, None, ["itmp1"], ["itmp2"])
            P.add("dve", lambda e: e.tensor_copy(iint[:], itmp[2][:]), ["itmp2"], ["iint"])
            P.add("dve", lambda e: e.tensor_copy(itmp[2][:], iint[:]), ["iint"], ["itmp2"])
            STT(itmp[1][:], itmp[2][:], -two_pi, itmp[1][:], ALU.mult, ALU.add, ["itmp2", "itmp1"], ["itmp1"])
            TS(itmp[2][:], itmp[1][:], math.pi, -two_pi, ALU.is_gt, ALU.mult, ["itmp1"], ["itmp2"])
            TT(itmp[1][:], itmp[1][:], itmp[2][:], ALU.add, ["itmp1", "itmp2"], ["itmp1"])
            TS(itmp[2][:], itmp[1][:], -math.pi, two_pi, ALU.is_lt, ALU.mult, ["itmp1"], ["itmp2"])
            TT(itmp[1][:], itmp[1][:], itmp[2][:], ALU.add, ["itmp1", "itmp2"], ["itmp1"])
            ACT(itmp[1][:], itmp[1][:], AF.Sin, ["itmp1"], ["itmp1"])
            if post_scalar_ap is not None:
                TS(dst, itmp[1][:], post_scalar_ap, post_imm, ALU.mult, ALU.mult, ["itmp1", "pidx"], ["tab"])
            else:
                P.add("dve", lambda e: e.tensor_copy(dst, itmp[1][:]), ["itmp1"], ["tab"])

        reduce_sin(SINS[:], 0.0, pidx[:, 4:5], -1.0)
        reduce_sin(COS[:], math.pi / 2, None, None)

    HG_KEYS = [("HT", t_) for t_ in range(NTB)] + [("gu", i_) for i_ in range(4)]

    def emit_mixer_weights_main(l, prefetch=False):
        ex = HG_KEYS if prefetch else []
        DMA("pool", w_in[:], win_d[l].rearrange("(k p) n -> p k n", p=128), (), ["w_in"] + ex)
        DMA("pool", w_q[:], wq_d[l].rearrange("(k p) n -> p k n", p=128), (), ["w_q"] + ex)
        DMA("pool", w_kv[:], wkv_d[l], (), ["w_kv"] + ex)
        DMA("pool", wo_mla[:], wout_d[l, 0:512, :].rearrange("(h p) n -> p h n", p=64), (), ["wo"] + ex)

    def emit_mixer_weights(l, have_main):
        if not have_main:
            emit_mixer_weights_main(l)
        DMA("pool", wo_mem[:], wout_d[l, 512:768, :].rearrange("(h p) n -> p h n", p=64), (), ["wo"])
        DMA("pool", wo_conv[:], wout_d[l, 768:1024, :].rearrange("(k p) n -> p k n", p=128), (), ["wo"])
        DMA("pool", wmem[:], wmem_d[l].rearrange("(k p) n -> p k n", p=128), (), ["wmem"])

    def emit_mem_path(s, l):
        MEMSET(Vaug[:, :, :, 64:65], 1.0, ["Vaug"])
        MEMSET(vmaug[:, :, :, 64:65], 1.0, ["vmaug"])
        DMA("sp", memtok[:], mem_d[s].rearrange("(j p) f -> p j f", p=128), (), ["memtok"])
        MEMSET(mss[:], 0.0, ["mss"])
        for j in range(2):
            ACT(msq[:], memtok[:, j, :], AF.Square, ["memtok"], ["msq", "mss"], accum=mss[:, j:j + 1])
        ACT(mss[:, 2:4], mss[:, 0:2], AF.Ln, ["mss"], ["mss"], scale=1.0 / D, bias=epsc[:, 0:1])
        ACT(mss[:, 4:6], mss[:, 2:4], AF.Exp, ["mss"], ["mss"], scale=-0.5)
        for j in range(2):
            ACT(memtok[:, j, :], memtok[:, j, :], AF.Copy, ["memtok", "mss"], ["memtok"], scale=mss[:, 4 + j:5 + j])
        for cp in range(4):
            b = PS.alloc()
            for ci in range(2):
                c = cp * 2 + ci
                for j in range(2):
                    TR(ps[b][:, ci * 256 + j * 128: ci * 256 + (j + 1) * 128], memtok[:, j, c * 128:(c + 1) * 128],
                       ["memtok", "ident"], [PK(b)])
            for ci in range(2):
                c = cp * 2 + ci
                TS(memT[:, c, :], ps[b][:, ci * 256:(ci + 1) * 256], gcol(l, G_MEM + c), None, ALU.mult, None,
                   [PK(b), "gains"], ["memT"])
            PS.release(b)
        for cc in range(2):
            b = PS.alloc()
            for k in range(KC):
                MM(ps[b][:, 0:MEMT], wmem[:, k, cc * 128:(cc + 1) * 128], memT[:, k, :], k == 0, k == KC - 1,
                   ["wmem", "memT"], [PK(b)])
            ACT(sq96[:, 0:MEMT], ps[b][:, 0:MEMT], AF.Square, [PK(b)], ["sq96"])
            b2 = PS.alloc()
            MM(ps[b2][:, 0:MEMT], blk64[:], sq96[:, 0:MEMT], True, True, ["blk64", "sq96"], [PK(b2)])
            RSTD(rstd[0][:, 0:MEMT], ps[b2][:, 0:MEMT], [PK(b2)], ["rstd0"])
            PS.release(b2)
            STT(kmT[:, cc, :], ps[b][:, 0:MEMT], gcol(l, G_KMEM), rstd[0][:, 0:MEMT], ALU.mult, ALU.mult,
                [PK(b), "rstd0", "gains"], ["kmT"])
            PS.release(b)
        for j in range(2):
            b = PS.alloc()
            for k in range(KC):
                MM(ps[b][:, 0:256], memT[:, k, j * 128:(j + 1) * 128], wmem[:, k, 256:512], k == 0, k == KC - 1,
                   ["wmem", "memT"], [PK(b)])
            ACT(vmaug[:, j, :, 0:64], ps[b][:, 0:256].rearrange("p (h d) -> p h d", h=NMH), AF.Copy,
                [PK(b)], ["vmaug"])
            PS.release(b)

    def emit_load_xt(s, l, t):
        c0 = t * TB
        if l == 0:
            DMA("sp", stage[:], x_d[s, c0:c0 + TB, :].rearrange("(j p) f -> p j f", p=128), (), ["R1", "R1b"])
            for c in range(KC):
                b = PS.alloc()
                for j in range(4):
                    TR(ps[b][:, j * 128:(j + 1) * 128], stage[:, j, c * 128:(c + 1) * 128], ["R1", "R1b", "ident"], [PK(b)])
                if c % 2 == 0:
                    ACT(xt[:, c, :], ps[b][:], AF.Copy, [PK(b)], [("xt", c)])
                else:
                    P.add("dve", lambda e, o=xt[:, c, :], i=ps[b][:]: e.tensor_copy(o, i), [PK(b)], [("xt", c)])
                PS.release(b)
        else:
            for c in range(KC):
                DMA("sp", xt[:, c, :], xs_d[s, c, :, c0:c0 + TB], (), [("xt", c)])

    XTK = [("xt", c) for c in range(KC)]

    def emit_m1(s, l, t):
        c0 = t * TB
        ACT(SQ[:], xt[:], AF.Square, XTK, ["R1"])
        b = PS.alloc()
        for k in range(KC):
            MM(ps[b][:], ones1024[:], SQ[:, k, :], k == 0, k == KC - 1, ["R1", "ones1024"], [PK(b)])
        if t == 0 and dump_on:
            P.add("dve", lambda e, o=t2[:], i_=ps[b][:]: e.tensor_copy(o, i_), [PK(b)], ["R2"])
            DUMP(17, t2[:], ["R2"])
        RSTD(rstd[0][:], ps[b][:], [PK(b)], ["rstd0"])
        PS.release(b)
        if t == 0:
            DUMP(14, SQ[:], ["R1"])
            DUMP(15, rstd[0][:], ["rstd0"])
            DUMP(16, xt[:], XTK)
        for k in range(KC):
            STT(hT[:, k, :], xt[:, k, :], gcol(l, G_MIX + k), rstd[0][:], ALU.mult, ALU.mult,
                [("xt", k), "rstd0", "gains"], [("hT", k), "R1b"])
        HTK = [("hT", k) for k in range(KC)]
        if t == 0:
            DUMP(0, hT[:], HTK)

        def inproj(col0, M):
            bb = PS.alloc()
            for k in range(KC):
                MM(ps[bb][0:M, :], w_in[:, k, col0:col0 + M], hT[:, k, :], k == 0, k == KC - 1,
                   ["w_in", ("hT", k), "R1b"], [PK(bb)])
            return bb

        WARM(cfg.get("warm_in", 32))
        for k2 in range(2):
            bb = inproj(k2 * 128, 128)
            ACT(qlat[:, k2, :], ps[bb][:], AF.Copy, [PK(bb)], ["R2"])
            ACT(sq2[:, k2, :], ps[bb][:], AF.Square, [PK(bb)], ["sq2"])
            PS.release(bb)
        b = PS.alloc()
        for k2 in range(2):
            MM(ps[b][:], ones256[:], sq2[:, k2, :], k2 == 0, k2 == 1, ["sq2", "ones256"], [PK(b)])
        RSTD(rstd[1][:], ps[b][:], [PK(b)], ["rstd1"])
        PS.release(b)
        for k2 in range(2):
            STT(qlatn[:, k2, :], qlat[:, k2, :], gcol(l, G_QLAT + k2), rstd[1][:], ALU.mult, ALU.mult,
                ["R2", "rstd1", "gains"], ["qlatn"])
        if t == 0:
            DUMP(1, qlatn[:], ["qlatn"])
        bb = inproj(256, 128)
        ACT(kvlat[:], ps[bb][:], AF.Copy, [PK(bb)], ["R2"])
        ACT(sq2[:, 0, :], ps[bb][:], AF.Square, [PK(bb)], ["sq2"])
        PS.release(bb)
        b = PS.alloc()
        MM(ps[b][:], ones128[:], sq2[:, 0, :], True, True, ["sq2", "ones128"], [PK(b)])
        RSTD(rstd[0][:], ps[b][:], [PK(b)], ["rstd0"])
        PS.release(b)
        STT(kvlatn[:], kvlat[:], gcol(l, G_KVLAT), rstd[0][:], ALU.mult, ALU.mult, ["R2", "rstd0", "gains"], ["kvlatn"])
        if t == 0:
            DUMP(2, kvlatn[:], ["kvlatn"])
        bA = inproj(320, 96)
        bB = inproj(1376, 96)
        kS, kR, kT1, kT2 = ("sq96", 1), ("r96", 1), ("t1", 1), ("t2", 1)
        ACT(sq96b[0:96, :], ps[bA][0:96, :], AF.Square, [PK(bA)], [kS])
        b = PS.alloc()
        MM(ps[b][0:96, :], blk96[0:96, 0:96], sq96b[0:96, :], True, True, [kS, "blk96"], [PK(b)])
        RSTD(r96b[64:96, :], ps[b][64:96, :], [PK(b)], [kR])
        PS.release(b)
        STT(t1b[64:96, :], ps[bA][64:96, :], gcol(l, G_K96, 64, 96), COS[64:96, c0:c0 + TB], ALU.mult, ALU.mult,
            [PK(bA), "COS", "gains"], [kT1])
        STT(t2b[64:96, :], ps[bB][64:96, :], gcol(l, G_K96SW, 64, 96), SINS[64:96, c0:c0 + TB], ALU.mult, ALU.mult,
            [PK(bB), "SINS", "gains"], [kT2])
        PS.release(bA)
        PS.release(bB)
        TT(t1b[64:96, :], t1b[64:96, :], t2b[64:96, :], ALU.add, [kT1, kT2], [kT1])
        TT(kfT[64:96, :, c0:c0 + TB], t1b[64:96, :].unsqueeze(1).to_broadcast([32, NH, TB]),
           r96b[64:96, :].unsqueeze(1).to_broadcast([32, NH, TB]), ALU.mult, [kT1, kR], [("kpe", t)])
        for cc in range(2):
            bb = inproj(416 + cc * 128, 128)
            ACT(sq2[:, cc, :], ps[bb][:], AF.Square, [PK(bb)], ["sq2"])
            b = PS.alloc()
            MM(ps[b][:], blk64[:], sq2[:, cc, :], True, True, ["sq2", "blk64"], [PK(b)])
            RSTD(rstd[cc][:], ps[b][:], [PK(b)], ["rstd%d" % cc])
            PS.release(b)
            STT(qm[:, cc, :], ps[bb][:], gcol(l, G_QMEM), rstd[cc][:], ALU.mult, ALU.mult,
                [PK(bb), "rstd%d" % cc, "gains"], ["qm"])
            PS.release(bb)
        if t == 0:
            MEMSET(vbuf[:, :, 0:2], 0.0, ["R3"])
        else:
            P.add("dve", lambda e: e.tensor_copy(vbuf[:, :, 0:2], vbuf[:, :, TB:TB + 2]), ["R3"], ["R3"])
        for cc in range(2):
            bgb = inproj(672 + cc * 128, 128)
            bgc = inproj(928 + cc * 128, 128)
            bu = inproj(1184 + cc * 128, 128)
            ACT(u_sb[:], ps[bu][:], AF.Copy, [PK(bu)], ["R3"])
            PS.release(bu)
            TT(vbuf[:, cc, 2:TB + 2], ps[bgc][:], u_sb[:], ALU.mult, [PK(bgc), "R3"], ["R3"])
            PS.release(bgc)
            ACT(ybuf[:], vbuf[:, cc, 2:TB + 2], AF.Copy, ["R3", "gains"], ["R3"], scale=gcol(l, G_CONVW + 2 * 2 + cc))
            STT(ybuf[:], vbuf[:, cc, 1:TB + 1], gcol(l, G_CONVW + 1 * 2 + cc), ybuf[:], ALU.mult, ALU.add,
                ["R3", "gains"], ["R3"])
            STT(ybuf[:], vbuf[:, cc, 0:TB], gcol(l, G_CONVW + 0 * 2 + cc), ybuf[:], ALU.mult, ALU.add,
                ["R3", "gains"], ["R3"])
            TT(oconv[:, cc, :], ps[bgb][:], ybuf[:], ALU.mult, [PK(bgb), "R3"], ["R3"])
            PS.release(bgb)
        ACT(sq2[:], oconv[:], AF.Square, ["R3"], ["sq2"])
        b = PS.alloc()
        for cc in range(2):
            MM(ps[b][:], ones256[:], sq2[:, cc, :], cc == 0, cc == 1, ["sq2", "ones256"], [PK(b)])
        RSTD(rstd[0][:], ps[b][:], [PK(b)], ["rstd0"])
        PS.release(b)
        for cc in range(2):
            STT(ocn[:, cc, :], oconv[:, cc, :], gcol(l, G_OCONV + cc), rstd[0][:], ALU.mult, ALU.mult,
                ["R3", "rstd0", "gains"], ["ocn"])
        wkv_v = w_kv[:, :].rearrange("p (h c) -> p h c", h=NH)[:, :, 64:128]
        for j in range(4):
            b = PS.alloc()
            MM(ps[b][:].rearrange("p (h d) -> p h d", h=NH), kvlatn[:, j * 128:(j + 1) * 128], wkv_v, True, True,
               ["w_kv", "kvlatn"], [PK(b)])
            ACT(Vaug[:, t * 4 + j, :, 0:64], ps[b][:].rearrange("p (h d) -> p h d", h=NH), AF.Copy,
                [PK(b)], [("Vaug", t)])
            PS.release(b)

        T1 = (t1, t1b)
        T2 = (t2, t2b)
        R96 = (r96, r96b)
        SQ96 = (sq96, sq96b)

        def prep_stages(h):
            pz = h % 2
            kT1, kT2, kR, kS = ("t1", pz), ("t2", pz), ("r96", pz), ("sq96", pz)
            bA = PS.alloc()
            bB = PS.alloc()
            bK = PS.alloc()
            for k2 in range(2):
                MM(ps[bA][0:96, :], w_q[:, k2, h * 96:(h + 1) * 96], qlatn[:, k2, :], k2 == 0, k2 == 1,
                   ["w_q", "qlatn"], [PK(bA)])
            for k2 in range(2):
                MM(ps[bB][0:96, :], w_q[:, k2, 768 + h * 96:768 + (h + 1) * 96], qlatn[:, k2, :], k2 == 0, k2 == 1,
                   ["w_q", "qlatn"], [PK(bB)])
            MM(ps[bK][0:64, :], w_kv[:, h * 128:h * 128 + 64], kvlatn[:], True, True, ["w_kv", "kvlatn"], [PK(bK)])
            yield
            ACT(SQ96[pz][0:96, :], ps[bA][0:96, :], AF.Square, [PK(bA)], [kS])
            ACT(sq2[0:64, pz, :], ps[bK][0:64, :], AF.Square, [PK(bK)], [("sq2", pz)])
            yield
            WARM(cfg.get("warm_stats", 0))
            b = PS.alloc()
            MM(ps[b][0:96, :], blk96[0:96, 0:96], SQ96[pz][0:96, :], True, True, [kS, "blk96"], [PK(b)])
            yield
            RSTD(R96[pz][0:96, :], ps[b][0:96, :], [PK(b), "R2"], [kR])
            PS.release(b)
            yield
            b2 = PS.alloc()
            MM(ps[b2][0:64, :], blk96[0:64, 0:64], sq2[0:64, pz, :], True, True, [("sq2", pz), "blk96"], [PK(b2)])
            STT(qf[0:64, h, :], ps[bA][0:64, :], gcol(l, G_Q96, 0, 64), R96[pz][0:64, :], ALU.mult, ALU.mult,
                [PK(bA), kR, "gains", "R1"], [("qf", h)])
            STT(T1[pz][64:96, :], ps[bA][64:96, :], gcol(l, G_Q96, 64, 96), COS[64:96, c0:c0 + TB], ALU.mult, ALU.mult,
                [PK(bA), "COS", "gains", "R2"], [kT1])
            PS.release(bA)
            yield
            RSTD(rstd[pz][0:64, :], ps[b2][0:64, :], [PK(b2)], ["rstd%d" % pz])
            PS.release(b2)
            STT(T2[pz][64:96, :], ps[bB][64:96, :], gcol(l, G_Q96SW, 64, 96), SINS[64:96, c0:c0 + TB], ALU.mult, ALU.mult,
                [PK(bB), "SINS", "gains", "R2"], [kT2])
            PS.release(bB)
            yield
            pq = "pool" if cfg.get("pool_rope", 0) else "dve"
            TT(T1[pz][64:96, :], T1[pz][64:96, :], T2[pz][64:96, :], ALU.add, [kT1, kT2], [kT1], q=pq)
            TT(qf[64:96, h, :], T1[pz][64:96, :], R96[pz][64:96, :], ALU.mult, [kT1, kR, "R1"], [("qf", h)], q=pq)
            STT(kfT[0:64, h, c0:c0 + TB], ps[bK][0:64, :], gcol(l, G_K96, 0, 64), rstd[pz][0:64, :],
                ALU.mult, ALU.mult, [PK(bK), "rstd%d" % pz, "gains"], [("kfT", t, h)])
            PS.release(bK)

        NSTAGE = 7

        sc_mla = 1.0 / math.sqrt(96.0)
        sc_mem = 1.0 / math.sqrt(64.0)
        VR = [("Vaug", tt) for tt in range(t + 1)]
        LA = cfg.get("LA", 2)
        pending = []
        deferred = []
        acc = {}
        ctr = [0]

        def tick():
            for dd in list(deferred):
                dd[0] -= 1
                if dd[0] <= 0:
                    deferred.remove(dd)
                    dd[1]()

        def front(blk):
            kind, h, kb, nkb = blk
            i = ctr[0]
            ctr[0] += 1
            bS = PS.alloc()
            pt = PT[i % 4]
            if kind == "mla":
                jl = kb - 4 * t
                q0 = jl * 128 if jl > 0 else 0
                MM(ps[bS][:, q0:TB], kfT[0:96, h, kb * 128:(kb + 1) * 128], qf[0:96, h, q0:TB], True, True,
                   [("kfT", kb // 4, h), ("kpe", kb // 4), ("qf", h), "R1"], [PK(bS)])
                ACT(pt[:, q0:TB], ps[bS][:, q0:TB], AF.Exp, [PK(bS)], [("PT", i % 4)], scale=sc_mla)
                if jl >= 0:
                    TT(pt[:, q0:q0 + 128], pt[:, q0:q0 + 128], cmask[:], ALU.mult, [("PT", i % 4), "cmask"],
                       [("PT", i % 4)])
            else:
                q0 = 0
                cc, r0 = h // 2, (h % 2) * 64
                MM(ps[bS][:, :], kmT[r0:r0 + 64, cc, kb * 128:(kb + 1) * 128], qm[r0:r0 + 64, cc, :], True, True,
                   ["kmT", "qm"], [PK(bS)])
                ACT(pt[:, :], ps[bS][:, :], AF.Exp, [PK(bS)], [("PT", i % 4)], scale=sc_mem)
            PS.release(bS)
            pending.append((blk, i, q0))

        def back():
            (kind, h, kb, nkb), i, q0 = pending.pop(0)
            pt = PT[i % 4]
            if kb == 0:
                acc[(kind, h)] = PS.alloc()
            bO = acc[(kind, h)]
            if kind == "mla":
                MM(ps[bO][0:65, q0:TB], Vaug[:, kb, h, :], pt[:, q0:TB], kb == 0, kb == nkb - 1,
                   VR + [("PT", i % 4)], [PK(bO)])
            else:
                MM(ps[bO][0:65, :], vmaug[:, kb, h, :], pt[:, :], kb == 0, kb == nkb - 1,
                   ["vmaug", ("PT", i % 4)], [PK(bO)])
            if kb == nkb - 1:
                del acc[(kind, h)]
                dk = ("drow", h % 2)
                dr = drow if h % 2 == 0 else rec2
                ACT(dr[64:65, :], ps[bO][64:65, :], AF.Copy, [PK(bO)], [dk])

                if cfg.get("bc_dma", 0):
                    pz_ = h % 2
                    rk = ("rcb", pz_)

                    def finA(dr=dr, dk=dk, pz_=pz_, rk=rk):
                        DMA("sp", rcb[pz_][0:64, :], bass.AP(dr, dr[64:65, :].offset, [[0, 64], [1, TB]]), [dk], [rk])

                    def finB(kind=kind, h=h, bO=bO, pz_=pz_, rk=rk):
                        dst, dkey = (omla, "R1b") if kind == "mla" else (omem, "omem")
                        P.add("dve", lambda e, o=rcb[pz_][0:64, :]: e.reciprocal(o, o), [rk], [rk])
                        TT(dst[0:64, h, :], ps[bO][0:64, :], rcb[pz_][0:64, :], ALU.mult, [PK(bO), rk], [dkey])
                        PS.release(bO)

                    deferred.append([cfg.get("finA", 1), finA])
                    deferred.append([cfg.get("finB", 4), finB])
                else:
                    def fin(kind=kind, h=h, bO=bO, dr=dr, dk=dk):
                        bD = PS.alloc()
                        MM(ps[bD][0:64, :], onesf[64:65, 0:64], dr[64:65, :], True, True, [dk, "onesf"], [PK(bD)])
                        dst, dkey = (omla, "R1b") if kind == "mla" else (omem, "omem")
                        P.add("dve", lambda e, o=rec[0:64, :], i_=ps[bD][0:64, :]: e.reciprocal(o, i_), [PK(bD)], ["rec"])
                        PS.release(bD)
                        TT(dst[0:64, h, :], ps[bO][0:64, :], rec[0:64, :], ALU.mult, [PK(bO), "rec"], [dkey])
                        PS.release(bO)

                    deferred.append([2, fin])

        def push(blk):
            front(blk)
            tick()
            if len(pending) > LA:
                back()

        def attn_head(h, gen):
            nkb = 4 * (t + 1)
            done = 0
            WARM(cfg.get("warm_head", 0))
            for kb in range(nkb):
                push(("mla", h, kb, nkb))
                if gen is not None:
                    if cfg.get("two_stage", 0):
                        want = 2 if kb < int(nkb * cfg.get("two_stage_frac", 0.5)) else NSTAGE
                    else:
                        span = max(1, int(nkb * cfg.get("prep_span", 0.01)))
                        want = min(NSTAGE, ((kb + 1) * NSTAGE + span - 1) // span)
                    while done < want:
                        if next(gen, "END") == "END":
                            done = NSTAGE
                            break
                        done += 1
            if gen is not None:
                for _ in gen:
                    pass

        if (t == 0 and cfg.get("prep_first", 0)) or cfg.get("prep_first_all", 0):
            nxt = 0
            active = []
            while nxt < NH or active:
                if nxt < NH and (not active or (len(active) < 2 and active[0][1] >= 3)):
                    active.append([prep_stages(nxt), 0])
                    nxt += 1
                for a_ in list(active):
                    if next(a_[0], "END") == "END":
                        active.remove(a_)
                    else:
                        a_[1] += 1
            for h in range(NH):
                attn_head(h, None)
        else:
            g0 = prep_stages(0)
            for _ in g0:
                pass
            WARM(cfg.get("warm_attn", 0))
            for h in range(NH):
                attn_head(h, prep_stages(h + 1) if h + 1 < NH else None)
        for h in range(NMH):
            for kb in range(2):
                push(("mem", h, kb, 2))
        while pending:
            back()
            tick()
        while deferred:
            tick()
        if t == 0:
            DUMP(3, qm[:], ["qm"])
            DUMP(4, ocn[:], ["ocn"])
            DUMP(5, qf[0:96, :, :], [("qf", h) for h in range(NH)])
            DUMP(6, kfT[0:96, :, 0:TB], [("kfT", 0, h) for h in range(NH)] + [("kpe", 0)])
            DUMP(7, Vaug[:, 0:4, :, :].rearrange("p a h d -> p a (h d)"), [("Vaug", 0)])
            DUMP(8, kmT[:], ["kmT"])
            DUMP(9, vmaug[:].rearrange("p a h d -> p a (h d)"), ["vmaug"])

    def emit_attention(s, l, t):
        return

    def emit_outproj(s, l, t, last_layer_no_ffn=False):
        c0 = t * TB
        if t == 0:
            DUMP(10, omla[0:64, :, :], ["R1b"])
            DUMP(11, omem[0:64, :, :], ["omem"])
        for (buf, key, nh, ones_m, gbase) in ((omla, "R1b", NH, ones512, G_OMLA), (omem, "omem", NMH, ones256, G_OMEM)):
            ACT(SQ[0:64, 0:nh, :], buf[0:64, :, :], AF.Square, [key], ["R1"])
            b = PS.alloc()
            for h in range(nh):
                MM(ps[b][0:64, :], ones_m[0:64, 0:64], SQ[0:64, h, :], h == 0, h == nh - 1, ["R1"], [PK(b)])
            RSTD(rstd[0][0:64, :], ps[b][0:64, :], [PK(b)], ["rstd0"])
            PS.release(b)
            for h in range(nh):
                STT(buf[0:64, h, :], buf[0:64, h, :], gcol(l, gbase + h, 0, 64), rstd[0][0:64, :], ALU.mult, ALU.mult,
                    [key, "rstd0", "gains"], [key])
        if t == 0:
            DUMP(12, omla[0:64, :, :], ["R1b"])
            DUMP(13, omem[0:64, :, :], ["omem"])
        WARM(cfg.get("warm_out", 32))
        for m in range(KC):
            b = PS.alloc()
            nmm = NH + NMH + 2
            i = 0
            for h in range(NH):
                MM(ps[b][:], wo_mla[0:64, h, m * 128:(m + 1) * 128], omla[0:64, h, :], i == 0, i == nmm - 1,
                   ["wo", "R1b"], [PK(b)])
                i += 1
            for h in range(NMH):
                MM(ps[b][:], wo_mem[0:64, h, m * 128:(m + 1) * 128], omem[0:64, h, :], i == 0, i == nmm - 1,
                   ["wo", "omem"], [PK(b)])
                i += 1
            for cc in range(2):
                MM(ps[b][:], wo_conv[:, cc, m * 128:(m + 1) * 128], ocn[:, cc, :], i == 0, i == nmm - 1,
                   ["wo", "ocn"], [PK(b)])
                i += 1
            TT(xt[:, m, :], xt[:, m, :], ps[b][:], ALU.add, [("xt", m), PK(b)], [("xt", m)])
            PS.release(b)
            DMA("sp", xs_d[s, m, :, c0:c0 + TB], xt[:, m, :], [("xt", m)], [("xs", s)])


    def emit_ffn(s, l, final, prefetch_l=None):
        for t in range(NTB):
            for c in range(KC):
                DMA("sp", XF[:, c, t * TB:(t + 1) * TB], xs_d[s, c, :, t * TB:(t + 1) * TB], [("xs", s)], [("XFl", c, t)])
        XFK = [("XF", c) for c in range(KC)]
        moe = (l % 2 == 1)
        skip = (n_units_cap == 0)
        if moe and not skip:
            DMA("sp", wr_sb[:], wr_d[0].rearrange("(k p) e -> p k e", p=128), (), ["wr_sb"])
            for k in range(KC):
                TS(wrg[:, k, :], wr_sb[:, k, :], gcol(l, G_FFN + k), None, ALU.mult, None, ["wr_sb", "gains"], ["wrg"])
        for t in range(0 if not skip else NTB, NTB):
            c0 = t * TB
            ACT(FSQ[:], XF[:, :, c0:c0 + TB], AF.Square, XFK + [("XFl", c, t) for c in range(KC)], ["FSQ"])
            b = PS.alloc()
            for k in range(KC):
                MM(ps[b][:], ones1024[:], FSQ[:, k, :], k == 0, k == KC - 1, ["FSQ", "ones1024"], [PK(b)])
            RSTD(frstd[:], ps[b][:], [PK(b)], ["frstd"])
            PS.release(b)
            for k in range(KC):
                STT(HT[:, k, c0:c0 + TB], XF[:, k, c0:c0 + TB], gcol(l, G_FFN + k), frstd[:], ALU.mult, ALU.mult,
                    [("XF", k), ("XFl", k, t), "frstd", "gains"], [("HT", t)])
            if moe:
                ACT(combT[32:33, c0:c0 + TB], frstd[32:33, :], AF.Copy, ["frstd"], ["rsrow"])

        rt_state = {"bT": None, "pend": []}
        RT = (rt, rt2, rt3)

        def router_transpose(jb):
            t_, j = jb // 4, jb % 4
            r_ = RT[jb % 3]
            rk = ("rt", jb % 3)
            if j == 0:
                rt_state["bT"] = PS.alloc()
            bT = rt_state["bT"]
            TR(ps[bT][0:8, j * 128:(j + 1) * 128], r_[:, 48:56], [rk, "ident"], [PK(bT)])
            if j == 3:
                ACT(combT[0:8, t_ * TB:(t_ + 1) * TB], ps[bT][0:8, :], AF.Copy, [PK(bT)], ["combT"])
                PS.release(bT)

        def router_block(jb):
            if len(rt_state["pend"]) >= 2:
                router_transpose(rt_state["pend"].pop(0))
            r_ = RT[jb % 3]
            rk = ("rt", jb % 3)
            tok = slice(jb * 128, (jb + 1) * 128)
            b = PS.alloc()
            for k in range(KC):
                MM(ps[b][:, 0:8], XF[:, k, tok], wrg[:, k, :], k == 0, k == KC - 1, [("XF", k), "wrg"], [PK(b)])
            MM(ps[b][:, 8:9], combT[32:33, tok], onesf[32:33, 0:1], True, True, ["rsrow", "onesf"], [PK(b)])
            ACT(r_[:, 8:9], ps[b][:, 8:9], AF.Copy, [PK(b)], [rk])
            TS(r_[:, 0:8], ps[b][:, 0:8], r_[:, 8:9], None, ALU.mult, None, [PK(b), rk], [rk])
            PS.release(b)
            P.add("dve", lambda e: e.max(r_[:, 16:24], r_[:, 0:8]), [rk], [rk])
            TS(r_[:, 24:32], r_[:, 0:8], r_[:, 16:17], None, ALU.subtract, None, [rk], [rk])
            ACT(r_[:, 24:32], r_[:, 24:32], AF.Exp, [rk], [rk])
            TS(r_[:, 32:40], r_[:, 0:8], r_[:, 17:18], None, ALU.is_ge, None, [rk], [rk])
            TT(r_[:, 24:32], r_[:, 24:32], r_[:, 32:40], ALU.mult, [rk], [rk])
            P.add("dve", lambda e: e.reduce_sum(r_[:, 40:41], r_[:, 24:32], AX.X), [rk], [rk])
            P.add("dve", lambda e: e.reciprocal(r_[:, 41:42], r_[:, 40:41]), [rk], [rk])
            TS(r_[:, 48:56], r_[:, 24:32], r_[:, 41:42], None, ALU.mult, None, [rk], [rk])
            rt_state["pend"].append(jb)

        def router_flush():
            while rt_state["pend"]:
                router_transpose(rt_state["pend"].pop(0))

        router_todo = list(range(S // 128)) if (moe and not skip) else []
        if not cfg.get("router_overlap", 1):
            while router_todo:
                router_block(router_todo.pop(0))
            router_flush()
        if not moe:
            li = l // 2
            units = [(wdgu_d[li], 0, 2816, wdd_d[li], 0, None), (wdgu_d[li], EFF, 2816 + EFF, wdd_d[li], EFF, None)]
        else:
            li = l // 2
            units = [(wegu_d[li, e], 0, EFF, wed_d[li, e], 0, e) for e in range(8)]
        units = units[:n_units_cap]
        gu_i = [0]
        dw_i = [0]
        HTK = [("HT", t) for t in range(NTB)]
        for ui, (wgu, gc0, uc0, wd, dr0, ex) in enumerate(units):
            def emit_cw(ex=ex):
                for t in range(NTB):
                    b = PS.alloc()
                    MM(ps[b][:], selT[0:8, ex, :], combT[0:8, t * TB:(t + 1) * TB], True, True, ["selT", "combT"], [PK(b)])
                    ACT(CW[:, t * TB:(t + 1) * TB], ps[b][:], AF.Copy, [PK(b)], ["CW"])
                    PS.release(b)

            cw_late = ex is not None and bool(router_todo)
            if ex is not None and not cw_late:
                emit_cw()
            for j in range(NJ):
                sl = gu_i[0] % NGU
                gu_i[0] += 1
                DMA("pool", gu[sl][:, :, 0:128],
                    wgu[:, gc0 + j * 128: gc0 + (j + 1) * 128].rearrange("(k p) n -> p k n", p=128), (), [("gu", sl)])
                DMA("pool", gu[sl][:, :, 128:256],
                    wgu[:, uc0 + j * 128: uc0 + (j + 1) * 128].rearrange("(k p) n -> p k n", p=128), (), [("gu", sl)])
                for t in range(NTB):
                    bg = PS.alloc()
                    bu = PS.alloc()
                    for k in range(KC):
                        MM(ps[bg][:], gu[sl][:, k, 0:128], HT[:, k, t * TB:(t + 1) * TB], k == 0, k == KC - 1,
                           [("gu", sl), ("HT", t)], [PK(bg)])
                    for k in range(KC):
                        MM(ps[bu][:], gu[sl][:, k, 128:256], HT[:, k, t * TB:(t + 1) * TB], k == 0, k == KC - 1,
                           [("gu", sl), ("HT", t)], [PK(bu)])
                    si = (j * NTB + t) % 2
                    ACT(ssb[si][:], ps[bg][:], AF.Silu, [PK(bg)], [("ssb", si)])
                    PS.release(bg)
                    TT(ABUF[:, j, t * TB:(t + 1) * TB], ssb[si][:], ps[bu][:], ALU.mult, [("ssb", si), PK(bu)],
                       [("A", j, t)])
                    PS.release(bu)
                for _ in range(2):
                    if router_todo:
                        router_block(router_todo.pop(0))
            while router_todo:
                router_block(router_todo.pop(0))
            router_flush()
            if cw_late:
                emit_cw()
            for m in range(KC):
                if m == NDW and prefetch_l is not None and ui == len(units) - 1:
                    emit_mixer_weights_main(prefetch_l, prefetch=True)
                sl = dw_i[0] % NDW
                dw_i[0] += 1
                DMA("pool", dw[sl][:], wd[dr0:dr0 + EFF, m * 128:(m + 1) * 128].rearrange("(j p) n -> p j n", p=128),
                    (), [("dw", sl)])
                for t in range(NTB):
                    b = PS.alloc()
                    for j in range(NJ):
                        MM(ps[b][:], dw[sl][:, j, :], ABUF[:, j, t * TB:(t + 1) * TB], j == 0, j == NJ - 1,
                           [("dw", sl), ("A", j, t)], [PK(b)])
                    xsl = XF[:, m, t * TB:(t + 1) * TB]
                    if ex is not None:
                        ti = (m * NTB + t) % 2
                        TT(tmpb[ti][:], ps[b][:], CW[:, t * TB:(t + 1) * TB], ALU.mult, [PK(b), "CW"], [("tmpb", ti)])
                        TT(xsl, xsl, tmpb[ti][:], ALU.add, [("XF", m), ("tmpb", ti)], [("XF", m)])
                    else:
                        TT(xsl, xsl, ps[b][:], ALU.add, [("XF", m), PK(b)], [("XF", m)])
                    PS.release(b)
        if final:
            outs = []
            for tk in range(S // 128):
                for half in range(2):
                    b = PS.alloc()
                    for ci in range(4):
                        c = half * 4 + ci
                        TR(ps[b][:, ci * 128:(ci + 1) * 128], XF[:, c, tk * 128:(tk + 1) * 128],
                           [("XF", c), ("XFl", c, tk // 4), "ident"], [PK(b)])
                    ost = ostages[tk % 4]
                    okey = ("ostage", tk % 4)
                    akeys = [("A", j, t) for j in range(2, 7) for t in range(NTB)]
                    if half == 0:
                        ACT(ost[:, 0:512], ps[b][:], AF.Copy, [PK(b)] + akeys, [okey])
                    else:
                        P.add("dve", lambda e, o=ost[:, 512:1024], i_=ps[b][:]: e.tensor_copy(o, i_),
                              [PK(b)] + akeys, [okey])
                    PS.release(b)
                outs.append(DMA("sp", y_d[s, tk * 128:(tk + 1) * 128, :], ostages[tk % 4][:], [("ostage", tk % 4)], [("y", s)]))
            return outs
        else:
            for c in range(KC):
                DMA("sp", xs_d[s, c, :, :], XF[:, c, :], [("XF", c)], [("xs", s)])
            return []

    have_main = [False]
    early = set()
    if do_mixer and cfg.get("early_w", 1):
        n0 = len(P.ops)
        emit_mixer_weights_main(0)
        early = set(P.ops[n0:])
        have_main[0] = True
    emit_init()
    P.fence(exclude=early)
    out_dmas = []
    for s in range(n_seq):
        for l in range(n_layers):
            if do_mixer:
                emit_mixer_weights(l, have_main[0])
                have_main[0] = False
                emit_mem_path(s, l)
                P.fence()
                for t in range(ntb_cap):
                    emit_load_xt(s, l, t)
                    emit_m1(s, l, t)
                    emit_attention(s, l, t)
                    emit_outproj(s, l, t)
            else:
                for t in range(NTB):
                    emit_load_xt(s, l, t)
                    for m in range(KC):
                        DMA("sp", xs_d[s, m, :, t * TB:(t + 1) * TB], xt[:, m, :], [("xt", m)], [("xs", s)])
            P.fence()
            final = (l == n_layers - 1)
            is_last = (s == n_seq - 1 and l == n_layers - 1)
            pf = None if (is_last or not do_mixer or not cfg.get("prefetch", 1) or n_units_cap == 0) else (l + 1) % n_layers
            out_dmas += emit_ffn(s, l, final, pf)
            have_main[0] = pf is not None
            P.fence()
    fin = P.add("sp", None, (), ())
    for o in out_dmas:
        fin.deps.add(o)
    P.finalize()

    sem_names = list(P.semcount.keys())
    import contextlib
    with contextlib.ExitStack() as st:
        sems = {nm: st.enter_context(nc.semaphore("s_" + nm)) for nm in sem_names}
        block = st.enter_context(nc.Block())

        @block.tensor
        def _(e):
            P.replay("pe", e, sems)

        @block.scalar
        def _(e):
            P.replay("act", e, sems)

        @block.vector
        def _(e):
            P.replay("dve", e, sems)

        @block.gpsimd
        def _(e):
            P.replay("pool", e, sems)

        @block.sync
        def _(e):
            P.replay("sp", e, sems)

    return nc, P


def prep_weights(inp):
    f = lambda a: np.ascontiguousarray(np.asarray(a, dtype=np.float32))
    w_in = f(inp["w_in"])
    kpe = w_in[:, :, 384:416]
    w_in_ext = np.concatenate([w_in, kpe[:, :, 16:32], kpe[:, :, 0:16]], axis=2)
    wq = f(inp["w_q_up"]).reshape(2, 256, NH, 96)
    ext = np.concatenate([wq[..., 0:64], wq[..., 80:96], wq[..., 64:80]], axis=-1)
    w_q_ext = np.concatenate([wq.reshape(2, 256, 768), ext.reshape(2, 256, 768)], axis=2)
    G = np.zeros((2, 128, NG), np.float32)
    for l in range(2):
        def colk(v, n):
            return np.asarray(v, np.float32).reshape(n, 128).T
        G[l, :, G_MIX:G_MIX + 8] = colk(inp["g_mix"][l], 8)
        G[l, :, G_FFN:G_FFN + 8] = colk(inp["g_ffn"][l], 8)
        G[l, :, G_QLAT:G_QLAT + 2] = colk(inp["g_q_lat"][l], 2)
        G[l, :, G_KVLAT] = inp["g_kv_lat"][l]
        gq = np.asarray(inp["g_q_mla"][l], np.float32)
        gk = np.asarray(inp["g_k_mla"][l], np.float32)
        G[l, 0:96, G_Q96] = gq
        G[l, 64:96, G_Q96SW] = np.concatenate([gq[80:96], gq[64:80]])
        G[l, 0:96, G_K96] = gk
        G[l, 64:96, G_K96SW] = np.concatenate([gk[80:96], gk[64:80]])
        G[l, :, G_QMEM] = np.tile(np.asarray(inp["g_q_mem"][l], np.float32), 2)
        G[l, :, G_KMEM] = np.tile(np.asarray(inp["g_k_mem"][l], np.float32), 2)
        go = np.asarray(inp["g_out"][l], np.float32)
        G[l, 0:64, G_OMLA:G_OMLA + 8] = go[0:512].reshape(8, 64).T
        G[l, 0:64, G_OMEM:G_OMEM + 4] = go[512:768].reshape(4, 64).T
        G[l, :, G_OCONV:G_OCONV + 2] = go[768:1024].reshape(2, 128).T
        cw = np.asarray(inp["conv_w"][l], np.float32)
        for tap in range(3):
            G[l, :, G_CONVW + tap * 2:G_CONVW + tap * 2 + 2] = cw[tap].reshape(2, 128).T
        G[l, :, G_MEM:G_MEM + 8] = colk(inp["g_mem"][l], 8)
    return {
        "w_in_ext": np.ascontiguousarray(w_in_ext),
        "w_q_ext": np.ascontiguousarray(w_q_ext),
        "w_kv_up": f(inp["w_kv_up"]),
        "w_mem_kv": f(inp["w_mem_kv"]),
        "w_out": f(inp["w_out"]),
        "w_dense_gu": f(inp["w_dense_gu"]),
        "w_dense_down": f(inp["w_dense_down"]),
        "w_router": f(inp["w_router"]),
        "w_expert_gu": f(inp["w_expert_gu"]),
        "w_expert_down": f(inp["w_expert_down"]),
        "gains": G,
    }


_CACHE = {}


def kernel(**inputs):
    x = np.asarray(inputs["x"], np.float32)
    mem = np.asarray(inputs["mem"], np.float32)
    shared = prep_weights(inputs)
    if "nc" not in _CACHE:
        _CACHE["nc"] = build_program()[0]
    nc = _CACHE["nc"]
    in_maps = []
    for c in range(NCORES):
        m = dict(shared)
        m["x"] = np.ascontiguousarray(x[c * SEQ_PER_CORE:(c + 1) * SEQ_PER_CORE])
        m["mem"] = np.ascontiguousarray(mem[c * SEQ_PER_CORE:(c + 1) * SEQ_PER_CORE])
        in_maps.append(m)
    res = run_bass_kernel_spmd(nc, in_maps, core_ids=list(range(NCORES)))
    out = np.concatenate([np.asarray(r["y"], np.float32) for r in res.results], axis=0)
    return out
```
